# Optimizing a Trainium2 kernel written in Bass

```python
import jax, jax.numpy as jnp
from jax import lax
import numpy as np

D_MODEL = 1024
BATCH = 8
SEQ = 4096
DEPTH = 1

D_MIX = D_MODEL
ATTN_HEAD_DIM = 64
ATTN_HEADS = (D_MIX // 2) // ATTN_HEAD_DIM
ATTN_KV_HEADS = 2
ATTN_WIDTH = ATTN_HEADS * ATTN_HEAD_DIM
ATTN_KV_WIDTH = ATTN_KV_HEADS * ATTN_HEAD_DIM
WINDOW = 128
BLOCK = 128
ROPE_THETA = 10000.0

GLA_HEADS = 4
GLA_WIDTH = D_MIX - ATTN_WIDTH
GLA_DV = GLA_WIDTH // GLA_HEADS
GLA_DK = GLA_DV // 2
GLA_QK_WIDTH = GLA_HEADS * GLA_DK
GLA_GATE_RANK = 16
GLA_GATE_TAU = 16.0
GLA_CHUNK = 64

D_FF = 2816
PLE_DIM = 256
EPS = 1e-6

IN_SIZES = (ATTN_WIDTH, ATTN_KV_WIDTH, ATTN_KV_WIDTH,
            GLA_QK_WIDTH, GLA_QK_WIDTH, GLA_WIDTH, GLA_GATE_RANK, GLA_WIDTH)
D_IN = sum(IN_SIZES)

kernel_name = "hymba_swa_sink_gla_macaron_ple"


def rmsnorm(x, g):
    xf = x.astype(jnp.float32)
    y = xf * lax.rsqrt(jnp.mean(xf * xf, axis=-1, keepdims=True) + EPS)
    return (y * g.astype(jnp.float32)).astype(x.dtype)


def swiglu(x, w_gate, w_up, w_down):
    return (jax.nn.silu(x @ w_gate) * (x @ w_up)) @ w_down


def rope(x, pos):
    dh = x.shape[-1]
    inv_freq = ROPE_THETA ** (-jnp.arange(0, dh, 2, dtype=jnp.float32) / dh)
    ang = pos.astype(jnp.float32)[:, None] * inv_freq[None, :]
    cos = jnp.cos(ang)[None, :, None, :]
    sin = jnp.sin(ang)[None, :, None, :]
    xf = x.astype(jnp.float32)
    x1, x2 = xf[..., : dh // 2], xf[..., dh // 2:]
    out = jnp.concatenate([x1 * cos - x2 * sin, x2 * cos + x1 * sin], axis=-1)
    return out.astype(x.dtype)


def split_cols(z):
    points = np.cumsum(IN_SIZES)[:-1].tolist()
    return jnp.split(z, points, axis=-1)


def sliding_window_attention(q, k, v, sinks):
    B, S, H, Dh = q.shape
    KVH = k.shape[2]
    G = H // KVH
    NB = S // BLOCK
    qb = q.reshape(B, NB, BLOCK, KVH, G, Dh)

    def band(t):
        tb = t.reshape(B, NB, BLOCK, KVH, Dh)
        prev = jnp.pad(tb[:, :-1], ((0, 0), (1, 0), (0, 0), (0, 0), (0, 0)))
        return jnp.concatenate([prev, tb], axis=2)

    kb, vb = band(k), band(v)
    scores = jnp.einsum('bnqhgd,bnkhd->bnhgqk', qb, kb).astype(jnp.float32) * (Dh ** -0.5)
    qi = jnp.arange(BLOCK)[:, None]
    kj = jnp.arange(2 * BLOCK)[None, :]
    rel = kj - BLOCK - qi
    in_band = (rel <= 0) & (rel > -WINDOW)
    kpos = jnp.arange(NB)[:, None, None] * BLOCK + kj[None] - BLOCK
    valid = in_band[None] & (kpos >= 0)
    scores = jnp.where(valid[None, :, None, None], scores, -jnp.inf)
    sink = sinks.astype(jnp.float32).reshape(KVH, G)[None, None, :, :, None, None]
    m = jnp.maximum(jnp.max(scores, axis=-1, keepdims=True), sink)
    e = jnp.exp(scores - m)
    denom = jnp.sum(e, axis=-1, keepdims=True) + jnp.exp(sink - m)
    probs = (e / denom).astype(v.dtype)
    out = jnp.einsum('bnhgqk,bnkhd->bnqhgd', probs, vb)
    return out.reshape(B, S, H * Dh)


def gla_chunked(q, k, v, log_alpha):
    B, S, H, DK = q.shape
    DV = v.shape[-1]
    C = GLA_CHUNK
    NC = S // C

    def chunks(t):
        return t.astype(jnp.float32).reshape(B, NC, C, H, t.shape[-1]).transpose(1, 0, 3, 2, 4)

    qc = chunks(q) * (DK ** -0.5)
    kc, vc, gc = chunks(k), chunks(v), chunks(log_alpha)
    causal = jnp.tril(jnp.ones((C, C), dtype=bool))[None, None, :, :, None]

    def step(state, inp):
        qi, ki, vi, gi = inp
        b = jnp.cumsum(gi, axis=2)
        b_last = b[:, :, -1:, :]
        diff = b[:, :, :, None, :] - b[:, :, None, :, :]
        decay = jnp.exp(jnp.where(causal, diff, -jnp.inf))
        attn = jnp.einsum('bhid,bhjd,bhijd->bhij', qi, ki, decay)
        o = (jnp.einsum('bhij,bhjv->bhiv', attn, vi)
             + jnp.einsum('bhid,bhdv->bhiv', qi * jnp.exp(b), state))
        new_state = (jnp.swapaxes(jnp.exp(b_last), 2, 3) * state
                     + jnp.einsum('bhjd,bhjv->bhdv', ki * jnp.exp(b_last - b), vi))
        return new_state, o

    s0 = jnp.zeros((B, H, DK, DV), jnp.float32)
    _, o = lax.scan(step, s0, (qc, kc, vc, gc))
    return o.transpose(1, 0, 3, 2, 4).reshape(B, S, H, DV).astype(v.dtype)


def setup_inputs(seed: int = 0) -> dict:
    key = jax.random.key(seed)
    ks = jax.random.split(key, 24)
    L, D, F = DEPTH, D_MODEL, D_FF

    def w(k, shape, fan_in):
        return jax.random.normal(k, shape, jnp.float32) * (fan_in ** -0.5)

    def gain(k, shape):
        return 1.0 + 0.05 * jax.random.normal(k, shape, jnp.float32)

    return {
        "x": jax.random.normal(ks[0], (BATCH, SEQ, D), jnp.float32),
        "p": jax.random.normal(ks[1], (L, BATCH, SEQ, PLE_DIM), jnp.float32),
        "ffn1_norm": gain(ks[2], (L, D)),
        "ffn1_w_gate": w(ks[3], (L, D, F), D),
        "ffn1_w_up": w(ks[4], (L, D, F), D),
        "ffn1_w_down": w(ks[5], (L, F, D), F),
        "mix_norm": gain(ks[6], (L, D)),
        "w_in": w(ks[7], (L, D, D_IN), D),
        "q_norm": gain(ks[8], (L, ATTN_HEAD_DIM)),
        "k_norm": gain(ks[9], (L, ATTN_HEAD_DIM)),
        "attn_sinks": 0.5 * jax.random.normal(ks[10], (L, ATTN_HEADS), jnp.float32),
        "gla_w_gate_up": w(ks[11], (L, GLA_GATE_RANK, GLA_QK_WIDTH), GLA_GATE_RANK),
        "gla_b_gate": 0.1 * jax.random.normal(ks[12], (L, GLA_QK_WIDTH), jnp.float32),
        "gla_out_norm": gain(ks[13], (L, GLA_DV)),
        "w_out": w(ks[14], (L, D_MIX, D), D_MIX),
        "ffn2_norm": gain(ks[15], (L, D)),
        "ffn2_w_gate": w(ks[16], (L, D, F), D),
        "ffn2_w_up": w(ks[17], (L, D, F), D),
        "ffn2_w_down": w(ks[18], (L, F, D), F),
        "ple_gate_norm": gain(ks[19], (L, D)),
        "ple_w_gate": w(ks[20], (L, D, D), D),
        "ple_w_proj": w(ks[21], (L, PLE_DIM, D), PLE_DIM),
        "ple_norm": gain(ks[22], (L, D)),
    }


def reference(x, p, ffn1_norm, ffn1_w_gate, ffn1_w_up, ffn1_w_down, mix_norm, w_in,
              q_norm, k_norm, attn_sinks, gla_w_gate_up, gla_b_gate, gla_out_norm, w_out,
              ffn2_norm, ffn2_w_gate, ffn2_w_up, ffn2_w_down,
              ple_gate_norm, ple_w_gate, ple_w_proj, ple_norm):
    B, S, _ = x.shape
    pos = jnp.arange(S)
    h = x
    for i in range(DEPTH):
        h = h + 0.5 * swiglu(rmsnorm(h, ffn1_norm[i]), ffn1_w_gate[i], ffn1_w_up[i], ffn1_w_down[i])

        n = rmsnorm(h, mix_norm[i])
        aq, ak, av, gq, gk, gv, glr, gog = split_cols(n @ w_in[i])

        aq = rope(rmsnorm(aq.reshape(B, S, ATTN_HEADS, ATTN_HEAD_DIM), q_norm[i]), pos)
        ak = rope(rmsnorm(ak.reshape(B, S, ATTN_KV_HEADS, ATTN_HEAD_DIM), k_norm[i]), pos)
        av = av.reshape(B, S, ATTN_KV_HEADS, ATTN_HEAD_DIM)
        attn_o = sliding_window_attention(aq, ak, av, attn_sinks[i])

        log_alpha = jax.nn.log_sigmoid(
            (glr @ gla_w_gate_up[i] + gla_b_gate[i]).astype(jnp.float32)) / GLA_GATE_TAU
        gla_o = gla_chunked(gq.reshape(B, S, GLA_HEADS, GLA_DK),
                            gk.reshape(B, S, GLA_HEADS, GLA_DK),
                            gv.reshape(B, S, GLA_HEADS, GLA_DV),
                            log_alpha.reshape(B, S, GLA_HEADS, GLA_DK))
        gla_o = rmsnorm(gla_o, gla_out_norm[i]) * jax.nn.silu(gog.reshape(B, S, GLA_HEADS, GLA_DV))
        gla_o = gla_o.reshape(B, S, GLA_WIDTH)

        h = h + jnp.concatenate([attn_o, gla_o], axis=-1) @ w_out[i]

        h = h + 0.5 * swiglu(rmsnorm(h, ffn2_norm[i]), ffn2_w_gate[i], ffn2_w_up[i], ffn2_w_down[i])

        gate = jax.nn.sigmoid(rmsnorm(h, ple_gate_norm[i]) @ ple_w_gate[i])
        emb = rmsnorm(p[i] @ ple_w_proj[i], ple_norm[i])
        h = h + gate * emb
    return h
```

```python
from contextlib import ExitStack
import numpy as np
import concourse.bass as bass
import concourse.mybir as mybir
from concourse.bass_utils import run_bass_kernel_spmd

F32 = mybir.dt.float32
BF16 = mybir.dt.bfloat16
AF = mybir.ActivationFunctionType
ALU = mybir.AluOpType

S_LEN = 4096
D = 1024
KC = 8
FF = 2816
FC = 22
TT = 512
NBLK = 4
EPS = 1e-6
RING = 11
PIECE = 2048


class Sched:
    def __init__(self, nc):
        self.nc = nc
        self.ops = []
        self.lw = {}
        self.rd = {}
        self.lane_last = {}
        self.lane_cnt = {}

    def add(self, eng, fn, reads=(), writes=(), lane=None):
        i = len(self.ops)
        me = ("lane", lane) if lane is not None else eng
        raw, other = set(), set()
        for k in reads:
            raw.update(self.lw.get(k, {}).values())
        for k in writes:
            other.update(self.lw.get(k, {}).values())
            other.update(self.rd.get(k, {}).values())
        if lane is not None and lane in self.lane_last:
            raw.add(self.lane_last[lane])
        deps = set()
        for d in raw | other:
            y = self.ops[d]
            if y["lane"] is None and lane is None and y["eng"] == eng:
                if eng == "pe" or d not in raw:
                    continue
            deps.add(d)
        for k in reads:
            self.rd.setdefault(k, {})[me] = i
        for k in writes:
            self.lw.setdefault(k, {})[me] = i
            self.rd[k] = {}
        op = dict(eng=eng, fn=fn, deps=deps, lane=lane, sig=False, val=None)
        if lane is not None:
            self.lane_last[lane] = i
            self.lane_cnt[lane] = self.lane_cnt.get(lane, 0) + 16
            op["val"] = self.lane_cnt[lane]
        self.ops.append(op)
        return i

    def emit(self, final_waits=()):
        nc = self.nc
        ops = self.ops
        for op in ops:
            for d in op["deps"]:
                ops[d]["sig"] = True
        for d in final_waits:
            ops[d]["sig"] = True
        cnt = {}
        for op in ops:
            if op["lane"] is None and op["sig"]:
                cnt[op["eng"]] = cnt.get(op["eng"], 0) + 1
                op["val"] = cnt[op["eng"]]
        engs = ["pe", "act", "dve", "pool", "sp"]
        with ExitStack() as st:
            esem = {e: st.enter_context(nc.semaphore("s_" + e)) for e in engs}
            lsem = {l: st.enter_context(nc.semaphore("l_%s" % (l,))) for l in self.lane_cnt}
            block = st.enter_context(nc.Block())

            def semof(op):
                return lsem[op["lane"]] if op["lane"] is not None else esem[op["eng"]]

            def run(ename):
                def body(eng):
                    waited = {}
                    for op in ops:
                        if op["eng"] != ename:
                            continue
                        need = {}
                        for d in op["deps"]:
                            y = ops[d]
                            s = semof(y)
                            if y["val"] > need.get(id(s), (None, 0))[1]:
                                need[id(s)] = (s, y["val"])
                        for sid, (s, v) in need.items():
                            if waited.get(sid, 0) < v:
                                eng.wait_ge(s, v)
                                waited[sid] = v
                        ins = op["fn"](eng)
                        if op["lane"] is not None:
                            ins.then_inc(lsem[op["lane"]], 16)
                        elif op["sig"]:
                            ins.then_inc(esem[ename], 1)
                    if ename == "sp":
                        for d in final_waits:
                            y = ops[d]
                            eng.wait_ge(semof(y), y["val"])
                return body

            block.tensor(run("pe"))
            block.scalar(run("act"))
            block.vector(run("dve"))
            block.gpsimd(run("pool"))
            block.sync(run("sp"))


COLS = dict(n1=0, nm=8, n2=16, npg=24, npl=32, qn=40, kn=41, gon=42, bg=43, sink=45)
NCOLS = 49
CF = dict(ident=0, glamask=128, scanmask=640)
NCF = 1152
CB = dict(ident=0, pswap=128, bones=256, ones=384, mdiag=512, mprev=1024)
NCB = 1536

Q_PERM = [0, 4, 1, 5, 2, 6, 3, 7]


def _fmtA(W, cols):
    K = W.shape[0]
    kc = K // 128
    out = np.zeros((128, 8, 256), np.float32)
    out[:, :kc, : len(cols)] = W[:, cols].reshape(kc, 128, len(cols)).transpose(1, 0, 2)
    return out.reshape(128, PIECE)


def _down_pieces(Wd):
    blocks = Wd.reshape(FC, 128, KC, 128).transpose(2, 0, 1, 3).reshape(KC * FC, 128, 128)
    pcs = blocks.reshape(11, 16, 128, 128).transpose(0, 2, 1, 3).reshape(11, 128, PIECE)
    return [pcs[i] for i in range(11)]


def _ffn_pieces(wg, wu, wd):
    out = []
    for i in range(11):
        cols = np.arange(256 * i, 256 * i + 256)
        out.append(_fmtA(wg, cols))
        out.append(_fmtA(wu, cols))
    out += _down_pieces(wd)
    return out


def _host_pack(inp):
    g = lambda k: np.asarray(inp[k], np.float32)[0]
    pieces = []
    pieces += _ffn_pieces(g("ffn1_w_gate"), g("ffn1_w_up"), g("ffn1_w_down"))
    w_in = g("w_in")
    qcols = np.concatenate([np.arange(h * 64, h * 64 + 64) for h in Q_PERM])
    pieces.append(_fmtA(w_in, qcols[:256]))
    pieces.append(_fmtA(w_in, qcols[256:]))
    pieces.append(_fmtA(w_in, np.arange(512, 768)))
    pieces.append(_fmtA(w_in, np.arange(1792, 1808)))
    pieces.append(_fmtA(w_in, np.arange(768, 1024)))
    pieces.append(_fmtA(w_in, np.arange(1024, 1280)))
    pieces.append(_fmtA(w_in, np.arange(1808, 2064)))
    pieces.append(_fmtA(w_in, np.arange(2064, 2320)))
    pieces.append(_fmtA(w_in, np.arange(1280, 1536)))
    pieces.append(_fmtA(w_in, np.arange(1536, 1792)))
    w_out = g("w_out")
    rows = np.concatenate([np.arange(h * 64, h * 64 + 64) for h in Q_PERM] + [np.arange(512, 1024)])
    w_out_p = w_out[rows]
    for i in range(4):
        pieces.append(_fmtA(w_out_p, np.arange(256 * i, 256 * i + 256)))
    pieces += _ffn_pieces(g("ffn2_w_gate"), g("ffn2_w_up"), g("ffn2_w_down"))
    wpg = g("ple_w_gate")
    for i in range(4):
        pieces.append(_fmtA(wpg, np.arange(256 * i, 256 * i + 256)))
    wpp = g("ple_w_proj")
    pieces.append(wpp.reshape(2, 128, 1024).transpose(1, 0, 2).reshape(128, PIECE))
    wstream = np.ascontiguousarray(np.stack(pieces, 0))

    cols = np.zeros((128, NCOLS), np.float32)
    for name, key in (("n1", "ffn1_norm"), ("nm", "mix_norm"), ("n2", "ffn2_norm"), ("npg", "ple_gate_norm"), ("npl", "ple_norm")):
        cols[:, COLS[name]: COLS[name] + 8] = g(key).reshape(8, 128).T
    cols[:, COLS["qn"]] = np.tile(g("q_norm"), 2)
    cols[:, COLS["kn"]] = np.tile(g("k_norm"), 2)
    cols[:, COLS["gon"]] = g("gla_out_norm")
    cols[:, COLS["bg"]: COLS["bg"] + 2] = g("gla_b_gate").reshape(2, 128).T
    sk = g("attn_sinks")
    for c in range(4):
        cols[:64, COLS["sink"] + c] = sk[c]
        cols[64:, COLS["sink"] + c] = sk[c + 4]

    cf = np.zeros((128, NCF), np.float32)
    cf[:, 0:128] = np.eye(128, dtype=np.float32)
    j = np.arange(128)[:, None]
    i = np.arange(128)[None, :]
    cf[:, 128:640] = np.tile((j <= i).astype(np.float32), (1, 4))
    sm = np.ones((128, 512), np.float32)
    sm[:, ::128] = 0.0
    cf[:, 640:1152] = sm

    cb = np.zeros((128, NCB), np.float32)
    cb[:, 0:128] = np.eye(128, dtype=np.float32)
    m = np.arange(128)
    swp = (m // 64) * 64 + ((m % 64) + 32) % 64
    cb[swp, 128 + m] = 1.0
    cb[:, 256:384] = ((j // 64) == (i // 64)).astype(np.float32)
    cb[:, 384:512] = 1.0
    NEG = -30000.0
    cb[:, 512:1024] = np.tile(np.where(j <= i, 0.0, NEG).astype(np.float32), (1, 4))
    cb[:, 1024:1536] = np.tile(np.where(j > i, 0.0, NEG).astype(np.float32), (1, 4))

    pos = np.arange(S_LEN).astype(np.float64)
    inv_freq = 10000.0 ** (-np.arange(0, 64, 2, dtype=np.float64) / 64)
    ang = pos[:, None] * inv_freq[None, :]
    cs = np.cos(ang).astype(np.float32).T
    sn = np.sin(ang).astype(np.float32).T
    cos2 = np.ascontiguousarray(np.tile(cs, (4, 1)))
    sin2s = np.ascontiguousarray(np.tile(np.concatenate([-sn, sn], 0), (2, 1)))
    return dict(wstream=wstream, cols=cols, cf=cf, cb=cb, cos2=cos2, sin2s=sin2s, wgu=g("gla_w_gate_up"))


def build_nc(n_tiles=S_LEN // TT, n_pieces=85, stop_after=None, mix_cut=99):
    nc = bass.Bass("TRN2", target_bir_lowering=False)
    dr = lambda n, s, k="ExternalInput": nc.dram_tensor(n, s, F32, kind=k).ap()
    x_d = dr("x", [S_LEN, D])
    p_d = dr("p", [S_LEN, 256])
    w_d = dr("wstream", [n_pieces, 128, PIECE])
    cols_d = dr("cols", [128, NCOLS])
    cf_d = dr("cf", [128, NCF])
    cb_d = dr("cb", [128, NCB])
    cos_d = dr("cos2", [128, S_LEN])
    sin_d = dr("sin2s", [128, S_LEN])
    wgu_d = dr("wgu", [16, 256])
    out_d = dr("out", [S_LEN, D], "ExternalOutput")

    with ExitStack() as st:
        sb = lambda n, s, d=F32: st.enter_context(nc.sbuf_tensor(n, s, d))
        S = Sched(nc)
        A = S.add

        PS = [st.enter_context(nc.psum_tensor("ps%d" % i, [128, 512], F32)) for i in range(8)]
        pk = lambda b: ("ps", b)
        hT = sb("hT", [128, KC, TT])
        nT = sb("nT", [128, KC, TT], BF16)
        actT = sb("actT", [128, FC, TT], BF16)
        ring = sb("ring", [128, RING, PIECE], BF16)
        xin = sb("xin", [128, 2, D])
        osb = sb("osb", [128, 2, D])
        pin = sb("pin", [128, NBLK, 256])
        pT = sb("pT", [128, 2, TT], BF16)
        sqb = sb("sqb", [128, 2, TT], BF16)
        sqe = sb("sqe", [128, 2, TT], BF16)
        tmpF = sb("tmpF", [128, 6, TT])
        tmpB = sb("tmpB", [128, 4, TT], BF16)
        qT = sb("qT", [128, 4, TT], BF16)
        kTa = sb("kTa", [128, 5 * 128], BF16)
        vtok = sb("vtok", [128, 5, 128], BF16)
        gvtok = sb("gvtok", [128, NBLK, 512], BF16)
        gqT = sb("gqT", [128, 2, TT], BF16)
        gkT = sb("gkT", [128, 2, TT], BF16)
        ktok = sb("ktok", [128, NBLK, 256], BF16)
        c16 = sb("c16", [128, 2, TT])
        E1 = sb("E1", [128, 2, TT])
        gsil = sb("gsil", [128, 4, TT], BF16)
        glrT = sb("glrT", [16, TT], BF16)
        mixT = sb("mixT", [128, KC, TT], BF16)
        Tst = sb("Tst", [128, 256])
        Ust = sb("Ust", [128, 256])
        Tbf4 = sb("Tbf4", [128, 5, 256], BF16)
        embF = sb("embF", [128, KC, TT])
        ppbuf = sb("ppbuf", [128, PIECE], BF16)
        cosT = sb("cosT", [128, TT])
        sinT = sb("sinT", [128, TT])
        cols = sb("colsb", [128, NCOLS])
        negb = sb("negb", [128, 2])
        sinke = sb("sinke", [128, 4])
        cf = sb("cfb", [128, NCF])
        cb = sb("cbb", [128, NCB], BF16)
        wgu = sb("wgub", [16, 256], BF16)

        ident = cf[:, 0:128]
        glamask = cf[:, 128:640]
        scanmask = cf[:, 640:1152]
        identb = cb[:, 0:128]
        pswap = cb[:, 128:256]
        bones = cb[:, 256:384]
        onesb = cb[:, 384:512]
        mdiag = cb[:, 512:1024]
        mprev = cb[:, 1024:1536]
        col = lambda name, i=0: cols[:, COLS[name] + i: COLS[name] + i + 1]

        class Rot:
            def __init__(self, t, n, name):
                self.t, self.n, self.name, self.i = t, n, name, 0

            def next(self):
                j = self.i % self.n
                self.i += 1
                return self.t[:, j, :], (self.name, j)

        rF = Rot(tmpF, 6, "tmpF")
        rB = Rot(tmpB, 4, "tmpB")
        rSq = Rot(sqb, 2, "sqb")

        def MM(out, lhsT, rhs, st_, sp_, r, w):
            A("pe", lambda e: e.matmul(out, lhsT, rhs, start=st_, stop=sp_), r, w)

        def TR(out, in_, idt, r, w):
            A("pe", lambda e: e.transpose(out, in_, idt), r, w)

        def ACT(out, in_, func, r, w, scale=None, bias=None):
            kw = {}
            if scale is not None:
                kw["scale"] = scale
            if bias is not None:
                kw["bias"] = bias
            A("act", lambda e: e.activation(out=out, in_=in_, func=func, **kw), r, w)

        def TTo(eng, out, in0, in1, op, r, w):
            A(eng, lambda e: e.tensor_tensor(out=out, in0=in0, in1=in1, op=op), r, w)

        def STT(out, in0, scalar, in1, op0, op1, r, w):
            A("dve", lambda e: e.scalar_tensor_tensor(out=out, in0=in0, scalar=scalar, in1=in1, op0=op0, op1=op1), r, w)

        def TS(eng, out, in0, s1, op0, r, w):
            A(eng, lambda e: e.tensor_scalar(out=out, in0=in0, scalar1=s1, scalar2=None, op0=op0), r, w)

        def DMA(q, out, in_, r, w, lane):
            return A(q, lambda e: e.dma_start(out=out, in_=in_), r, w, lane=lane)

        class WStream:
            def __init__(self):
                self.issued = 0
                self.acq = 0
                self.released = 0
                self.total = n_tiles * (n_pieces - 1)
                self.done = {}
                self.pinned = set()

            def pump(self):
                while self.done.get(self.released, False):
                    self.released += 1
                while self.issued < min(self.total, self.released + RING):
                    g = self.issued
                    slot = g % RING
                    pc = g % (n_pieces - 1)
                    DMA("pool", ring[:, slot, :], w_d[pc], (), [("ring", slot)], lane=("w", slot))
                    self.issued += 1

            def acquire(self, pin=False):
                g = self.acq
                self.acq += 1
                self.pump()
                assert g < self.issued, (g, self.issued, self.released)
                if pin:
                    self.pinned.add(g)
                self.last = g
                slot = g % RING
                return ring[:, slot, :], ("ring", slot)

            def release(self, n=1):
                g = self.released
                while n > 0:
                    assert g < self.acq
                    if not self.done.get(g, False) and g not in self.pinned:
                        self.done[g] = True
                        n -= 1
                    g += 1
                self.pump()

            def unpin(self, g):
                self.pinned.discard(g)
                self.done[g] = True
                self.pump()

        W = WStream()

        DMA("sp", cols[:], cols_d, (), ["cols"], "c0")
        DMA("sp", cf[:], cf_d, (), ["cf"], "c1")
        DMA("pool", cb[:], cb_d, (), ["cb"], "c2")
        DMA("pool", wgu[:], wgu_d, (), ["wgu"], "c3")
        TS("dve", negb[:], cols[:, COLS["bg"]: COLS["bg"] + 2], -1.0, ALU.mult, ["cols"], ["negb"])
        ACT(sinke[:], cols[:, COLS["sink"]: COLS["sink"] + 4], AF.Exp, ["cols"], ["sinke"])
        A("dve", lambda e: e.memset(Tst[:], 0.0), (), ["Tst"])
        A("dve", lambda e: e.memset(Tbf4[:, 0, :], 0.0), (), [("Tbf", 0)])

        def rms_feature_major(src_keys, src_aps, gname, nchunks, dim):
            for kc in range(nchunks):
                sq, sqk = rSq.next()
                if gname == "n1":
                    TTo("dve", sq, src_aps[kc], src_aps[kc], ALU.mult, [src_keys[kc]], [sqk])
                else:
                    ACT(sq, src_aps[kc], AF.Square, [src_keys[kc]], [sqk])
                MM(PS[6][:], onesb, sq, kc == 0, kc == nchunks - 1, [sqk, "cb"], [pk(6)])
            t, tk = rF.next()
            ACT(t, PS[6][:], AF.Ln, [pk(6)], [tk], scale=1.0 / dim, bias=EPS)
            ACT(t, t, AF.Exp, [tk], [tk], scale=-0.5)
            return t, tk

        def norm_to_nT(gname):
            rstd, rk = rms_feature_major([("hT", kc) for kc in range(KC)], [hT[:, kc, :] for kc in range(KC)], gname, KC, D)
            for kc in range(KC):
                STT(nT[:, kc, :], hT[:, kc, :], col(gname, kc), rstd, ALU.mult, ALU.mult,
                    [("hT", kc), rk, "cols"], [("nT", kc)])

        def ffn(gname, extra=None):
            norm_to_nT(gname)
            for i in range(11):
                pg, pgk = W.acquire()
                pu, puk = W.acquire()
                pg3 = pg.rearrange("p (k c) -> p k c", k=8)
                pu3 = pu.rearrange("p (k c) -> p k c", k=8)
                if i == 0:
                    for kc in range(KC):
                        for fl in range(2):
                            bg_, bu_ = (0, 1) if fl == 0 else (2, 3)
                            MM(PS[bg_][:], pg3[:, kc, fl * 128:(fl + 1) * 128], nT[:, kc, :], kc == 0, kc == KC - 1,
                               [pgk, ("nT", kc)], [pk(bg_)])
                            MM(PS[bu_][:], pu3[:, kc, fl * 128:(fl + 1) * 128], nT[:, kc, :], kc == 0, kc == KC - 1,
                               [puk, ("nT", kc)], [pk(bu_)])
                for fl in range(2):
                    fc = 2 * i + fl
                    bg_, bu_ = (0, 1) if fc % 2 == 0 else (2, 3)
                    for kc in range(KC if i > 0 else 0):
                        MM(PS[bg_][:], pg3[:, kc, fl * 128:(fl + 1) * 128], nT[:, kc, :], kc == 0, kc == KC - 1,
                           [pgk, ("nT", kc)], [pk(bg_)])
                    for kc in range(KC if i > 0 else 0):
                        MM(PS[bu_][:], pu3[:, kc, fl * 128:(fl + 1) * 128], nT[:, kc, :], kc == 0, kc == KC - 1,
                           [puk, ("nT", kc)], [pk(bu_)])
                    sg, sgk = rF.next()
                    ACT(sg, PS[bg_][:], AF.Silu, [pk(bg_)], [sgk])
                    TTo("dve", actT[:, fc, :], sg, PS[bu_][:], ALU.mult, [sgk, pk(bu_)], [("act", fc)])
                W.release(2)
                if extra is not None:
                    extra(i)
            cur = None
            for dc in range(KC):
                b = 4 + dc % 2
                for fc in range(FC):
                    bi = dc * FC + fc
                    if bi % 16 == 0:
                        if cur is not None:
                            W.release()
                        cur = W.acquire()
                    pd, pdk = cur
                    o = (bi % 16) * 128
                    MM(PS[b][:], pd[:, o:o + 128], actT[:, fc, :], fc == 0, fc == FC - 1, [pdk, ("act", fc)], [pk(b)])
                STT(hT[:, dc, :], PS[b][:], 0.5, hT[:, dc, :], ALU.mult, ALU.add, [pk(b), ("hT", dc)], [("hT", dc)])
            W.release()

        def issue_x(t, blk):
            r0 = t * TT + blk * 128
            xb = blk % 2
            DMA("sp", xin[:, xb, :], x_d[r0:r0 + 128, :], (), [("xin", xb)], ("xin", xb))

        def load_tile(t):
            if t == 0:
                issue_x(0, 0)
                issue_x(0, 1)
            DMA("sp", cosT[:], cos_d[:, t * TT:(t + 1) * TT], (), ["cosT"], "cos")
            DMA("sp", sinT[:], sin_d[:, t * TT:(t + 1) * TT], (), ["sinT"], "sin")
            for blk in range(NBLK):
                xb = blk % 2
                for half in range(2):
                    b = (7, 6, 2, 3)[(blk % 2) * 2 + half]
                    for kl in range(4):
                        kc = half * 4 + kl
                        TR(PS[b][:, kl * 128:(kl + 1) * 128], xin[:, xb, kc * 128:(kc + 1) * 128], ident,
                           [("xin", xb), "cf"], [pk(b)])
                    A("act", lambda e, b=b, half=half, blk=blk: e.copy(
                        hT[:, half * 4:half * 4 + 4, blk * 128:(blk + 1) * 128],
                        PS[b][:].rearrange("p (k c) -> p k c", k=4)),
                      [pk(b)], [("hT", half * 4 + kl) for kl in range(4)])
                if blk < 2:
                    issue_x(t, blk + 2)
                elif t + 1 < n_tiles:
                    issue_x(t + 1, blk - 2)
            for blk in range(NBLK):
                r0 = t * TT + blk * 128
                DMA("sp", pin[:, blk, :], p_d[r0:r0 + 128, :], (), [("pin", blk)], ("pin", blk))

        out_ops = []

        def store_tile(t):
            mix32 = mixT[:].rearrange("p k t -> p (k t)").bitcast(F32).rearrange("p (s d) -> p s d", s=2)
            for blk in range(NBLK):
                r0 = t * TT + blk * 128
                if blk < 2:
                    stage = osb[:, blk, :]
                    keys = [[("osb", blk, 0)], [("osb", blk, 1)]]
                else:
                    stage = mix32[:, blk - 2, :]
                    kk = [("mixA" if blk == 2 else "mixG", b_) for b_ in range(NBLK)]
                    keys = [kk, kk]
                for half in range(2):
                    b = (7, 6, 2, 3)[(blk % 2) * 2 + half]
                    for kl in range(4):
                        kc = half * 4 + kl
                        TR(PS[b][:, kl * 128:(kl + 1) * 128], hT[:, kc, blk * 128:(blk + 1) * 128], ident,
                           [("hT", kc), "cf"], [pk(b)])
                    A("act", lambda e, b=b, half=half, stage=stage: e.copy(stage[:, half * 512:(half + 1) * 512], PS[b][:]),
                      [pk(b)], keys[half])
                out_ops.append(DMA("sp", out_d[r0:r0 + 128, :], stage, keys[0] + keys[1], (), ("out", blk)))

        def qk_A(piece3, pkey, cl, gname, bset, skip_mm=False):
            bx = bset[0]
            for kc in range(0 if skip_mm else KC):
                MM(PS[bx][:], piece3[:, kc, cl * 128:(cl + 1) * 128], nT[:, kc, :], kc == 0, kc == KC - 1,
                   [pkey, ("nT", kc)], [pk(bx)])
            sq, sqk = rSq.next()
            ACT(sq, PS[bx][:], AF.Square, [pk(bx)], [sqk])
            xg, xgk = rB.next()
            ACT(xg, PS[bx][:], AF.Identity, [pk(bx), "cols"], [xgk], scale=col(gname))
            return sq, sqk, xg, xgk

        def qk_B(stt, bset, dst, dstk):
            sq, sqk, xg, xgk = stt
            bs, bw = bset[1], bset[2]
            MM(PS[bs][:], bones, sq, True, True, [sqk, "cb"], [pk(bs)])
            MM(PS[bw][:], pswap, xg, True, True, [xgk, "cb"], [pk(bw)])
            rs, rsk = rF.next()
            ACT(rs, PS[bs][:], AF.Ln, [pk(bs)], [rsk], scale=1.0 / 64, bias=EPS)
            ACT(rs, rs, AF.Exp, [rsk], [rsk], scale=-0.5)
            t1, t1k = rF.next()
            TTo("pool", t1, xg, cosT[:], ALU.mult, [xgk, "cosT"], [t1k])
            t2, t2k = rF.next()
            TTo("dve", t2, PS[bw][:], sinT[:], ALU.mult, [pk(bw), "sinT"], [t2k])
            TTo("dve", t2, t2, t1, ALU.add, [t2k, t1k], [t2k])
            TTo("dve", dst, t2, rs, ALU.mult, [t2k, rsk], [dstk])

        def mixer(t):
            norm_to_nT("nm")
            bsets = [(0, 1, 2), (3, 4, 5)]
            pq0, pq0k = W.acquire()
            pq03 = pq0.rearrange("p (k c) -> p k c", k=8)
            for kc in range(KC):
                for cl in range(2):
                    MM(PS[bsets[cl][0]][:], pq03[:, kc, cl * 128:(cl + 1) * 128], nT[:, kc, :], kc == 0, kc == KC - 1,
                       [pq0k, ("nT", kc)], [pk(bsets[cl][0])])
            st0 = qk_A(pq03, pq0k, 0, "qn", bsets[0], skip_mm=True)
            st1 = qk_A(pq03, pq0k, 1, "qn", bsets[1], skip_mm=True)
            W.release()
            qk_B(st0, bsets[0], qT[:, 0, :], ("qT", 0))
            pq1, pq1k = W.acquire()
            pq13 = pq1.rearrange("p (k c) -> p k c", k=8)
            st2 = qk_A(pq13, pq1k, 0, "qn", bsets[0])
            qk_B(st1, bsets[1], qT[:, 1, :], ("qT", 1))
            st3 = qk_A(pq13, pq1k, 1, "qn", bsets[1])
            W.release()
            qk_B(st2, bsets[0], qT[:, 2, :], ("qT", 2))
            pa, pak = W.acquire()
            pa3 = pa.rearrange("p (k c) -> p k c", k=8)
            st4 = qk_A(pa3, pak, 0, "kn", bsets[0])
            qk_B(st3, bsets[1], qT[:, 3, :], ("qT", 3))
            for blk in range(NBLK):
                for kc in range(KC):
                    MM(PS[6][:, blk * 128:(blk + 1) * 128], nT[:, kc, blk * 128:(blk + 1) * 128], pa3[:, kc, 128:256],
                       kc == 0, kc == KC - 1, [pak, ("nT", kc)], [pk(6)])
            A("act", lambda e: e.copy(vtok[:, 1:5, :], PS[6][:].rearrange("p (b c) -> p b c", b=4)), [pk(6)], ["vtok"])
            W.release()
            qk_B(st4, bsets[0], kTa[:, 128:640], "kT")
            if mix_cut <= 2:
                return
            pl, plk = W.acquire()
            pl3 = pl.rearrange("p (k c) -> p k c", k=8)
            for kc in range(KC):
                MM(PS[2][0:16, :], pl3[:, kc, 0:16], nT[:, kc, :], kc == 0, kc == KC - 1, [plk, ("nT", kc)], [pk(2)])
            A("act", lambda e: e.copy(glrT[:], PS[2][0:16, :]), [pk(2)], ["glrT"])
            W.release()
            pgq, pgqk = W.acquire()
            pgk_, pgkk = W.acquire()
            pgq3 = pgq.rearrange("p (k c) -> p k c", k=8)
            pgk3 = pgk_.rearrange("p (k c) -> p k c", k=8)
            for P in range(2):
                MM(PS[3][:], wgu[:, P * 128:(P + 1) * 128], glrT[:], True, True, ["wgu", "glrT"], [pk(3)])
                sp_, spk = rF.next()
                ACT(sp_, PS[3][:], AF.Exp, [pk(3), "negb"], [spk], scale=-1.0, bias=negb[:, P:P + 1])
                ACT(sp_, sp_, AF.Ln, [spk], [spk], bias=1.0)
                A("dve", lambda e, P=P, sp_=sp_: e.tensor_tensor_scan(out=c16[:, P, :], data0=scanmask, data1=sp_, initial=0.0,
                                                                    op0=ALU.mult, op1=ALU.add),
                  [spk, "cf"], [("c16", P)])
                ACT(E1[:, P, :], c16[:, P, :], AF.Exp, [("c16", P)], [("E1", P)], scale=-1.0 / 16)
                e2, e2k = rF.next()
                ACT(e2, c16[:, P, :], AF.Exp, [("c16", P)], [e2k], scale=1.0 / 16)
                for kc in range(KC):
                    MM(PS[0][:], pgq3[:, kc, P * 128:(P + 1) * 128], nT[:, kc, :], kc == 0, kc == KC - 1,
                       [pgqk, ("nT", kc)], [pk(0)])
                STT(gqT[:, P, :], PS[0][:], 0.125, E1[:, P, :], ALU.mult, ALU.mult, [pk(0), ("E1", P)], [("gqT", P)])
                for kc in range(KC):
                    MM(PS[1][:], pgk3[:, kc, P * 128:(P + 1) * 128], nT[:, kc, :], kc == 0, kc == KC - 1,
                       [pgkk, ("nT", kc)], [pk(1)])
                TTo("dve", gkT[:, P, :], PS[1][:], e2, ALU.mult, [pk(1), e2k], [("gkT", P)])
            W.release(2)
            ps7b = PS[7][:].bitcast(BF16)
            for blk in range(NBLK):
                for P in range(2):
                    o = blk * 256 + P * 128
                    TR(ps7b[:, o:o + 128], gkT[:, P, blk * 128:(blk + 1) * 128], identb, [("gkT", P), "cb"], [pk(7)])
            A("act", lambda e: e.copy(ktok[:].rearrange("p b c -> p (b c)"), ps7b), [pk(7)], ["ktok"])
            for hg in range(2):
                po, pok = W.acquire()
                po3 = po.rearrange("p (k c) -> p k c", k=8)
                for hl in range(2):
                    h = hg * 2 + hl
                    b = 4 + hl
                    for kc in range(KC):
                        MM(PS[b][:], po3[:, kc, hl * 128:(hl + 1) * 128], nT[:, kc, :], kc == 0, kc == KC - 1,
                           [pok, ("nT", kc)], [pk(b)])
                    ACT(gsil[:, h, :], PS[b][:], AF.Silu, [pk(b)], [("gsil", h)])
                W.release()
            gv_state = {}

            def gv_unit(u):
                hv, bp = u // 2, u % 2
                if bp == 0:
                    gv_state["p"] = W.acquire()
                pv, pvk = gv_state["p"]
                pv3 = pv.rearrange("p (k c) -> p k c", k=8)
                b = 2 + bp
                for bl in range(2):
                    blk_ = bp * 2 + bl
                    for kc in range(KC):
                        MM(PS[b][:, bl * 256:(bl + 1) * 256], nT[:, kc, blk_ * 128:(blk_ + 1) * 128], pv3[:, kc, :],
                           kc == 0, kc == KC - 1, [pvk, ("nT", kc)], [pk(b)])
                A("act", lambda e, b=b, bp=bp, hv=hv: e.copy(
                    gvtok[:, bp * 2:bp * 2 + 2, hv * 256:(hv + 1) * 256],
                    PS[b][:].rearrange("p (b c) -> p b c", b=2)), [pk(b)], [("gvtok", hv, bp)])
                if bp == 1:
                    W.release()

            for blk in range(NBLK):
                first = (t == 0 and blk == 0)
                seq = ["cur"] if first else ["prev", "cur"]
                es = {}
                for j in range(2):
                    pr = slice(64 * j, 64 * j + 64)
                    for which in seq:
                        kb = blk if which == "prev" else blk + 1
                        bS = 4 + 2 * j + (0 if which == "prev" else 1)
                        MM(PS[bS][:], kTa[pr, kb * 128:(kb + 1) * 128], qT[pr, :, blk * 128:(blk + 1) * 128], True, False,
                           ["kT", "kTprev"] + [("qT", c) for c in range(4)], [pk(bS)])
                        MM(PS[bS][:], identb, mprev if which == "prev" else mdiag, False, True, ["cb"], [pk(bS)])
                        e_, ek = rB.next()
                        ACT(e_, PS[bS][:], AF.Exp, [pk(bS)], [ek], scale=0.125)
                        es[(j, which)] = (e_, ek)
                gv_unit(blk)
                for j in range(2):
                    pr = slice(64 * j, 64 * j + 64)
                    for n_, which in enumerate(seq):
                        vb = blk if which == "prev" else blk + 1
                        e_, ek = es[(j, which)]
                        MM(PS[0][pr, :], vtok[:, vb, 64 * j:64 * j + 64], e_, n_ == 0, n_ == len(seq) - 1,
                           [ek, "vtok", "vprev"], [pk(0)])
                    for n_, which in enumerate(seq):
                        e_, ek = es[(j, which)]
                        MM(PS[1][pr, :], onesb[:, 0:64], e_, n_ == 0, n_ == len(seq) - 1, [ek, "cb"], [pk(1)])
                ld, ldk = rF.next()
                for c in range(4):
                    ACT(ld[:, c * 128:(c + 1) * 128], PS[1][:, c * 128:(c + 1) * 128], AF.Ln, [pk(1), "sinke"], [ldk],
                        bias=sinke[:, c:c + 1])
                ACT(ld, ld, AF.Exp, [ldk], [ldk], scale=-1.0)
                TTo("dve", mixT[:, 0:4, blk * 128:(blk + 1) * 128], PS[0][:].rearrange("p (c q) -> p c q", c=4),
                    ld.rearrange("p (c q) -> p c q", c=4), ALU.mult, [pk(0), ldk], [("mixA", blk)])
            if mix_cut <= 3:
                return
            A("pool", lambda e: e.tensor_copy(out=kTa[:, 0:128], in_=kTa[:, 512:640]), ["kT"], ["kTprev"])
            A("pool", lambda e: e.tensor_copy(out=vtok[:, 0, :], in_=vtok[:, 4, :]), ["vtok"], ["vprev"])

            if mix_cut <= 5:
                return
            if mix_cut <= 6:
                return
            gvk = [("gvtok", hv, bp) for hv in range(2) for bp in range(2)]
            ams = []
            mixk = [("mixA", b) for b in range(NBLK)] + [("mixG", b) for b in range(NBLK)]
            mixak = [("mixA", b) for b in range(NBLK)]
            WO_BANKS = [1, 3]
            OT_BANKS = [2, 0, 4, 5]
            wo_state = {}

            def wout_pass_a():
                wo_state["p"] = [W.acquire()]
                for dc in range(2):
                    pwa, pwak = wo_state["p"][dc // 2]
                    pwa3 = pwa.rearrange("p (k c) -> p k c", k=8)
                    b = WO_BANKS[dc]
                    dl = dc % 2
                    for kc in range(4):
                        MM(PS[b][:], pwa3[:, kc, dl * 128:(dl + 1) * 128], mixT[:, kc, :], kc == 0, False,
                           [pwak] + mixak, [pk(b)])

            for blk in range(NBLK):
                bc = slice(blk * 128, (blk + 1) * 128)
                bev, bod = (2, 6) if blk % 2 == 0 else (4, 5)
                for h in range(4):
                    pr = slice(64 * (h % 2), 64 * (h % 2) + 64)
                    P = h // 2
                    bsc = bev if h % 2 == 0 else bod
                    MM(PS[bsc][:, P * 128:(P + 1) * 128], gkT[pr, P, bc], gqT[pr, P, bc], True, True,
                       [("gkT", P), ("gqT", P)], [pk(bsc)])
                am, amk = rB.next()
                am4 = am.rearrange("p (a r q) -> p a r q", a=2, r=2)
                for r_ in range(2):
                    bsc = bev if r_ == 0 else bod
                    TTo("dve", am4[:, :, r_, :], PS[bsc][:, 0:256].rearrange("p (a q) -> p a q", a=2),
                        glamask[:, 0:256].rearrange("p (a q) -> p a q", a=2), ALU.mult, [pk(bsc), "cf"], [amk])
                ams.append((am, amk))
                bd = 3 if blk < 2 else 7
                for h in range(4):
                    pr = slice(64 * (h % 2), 64 * (h % 2) + 64)
                    P = h // 2
                    o = (blk % 2) * 256 + P * 128
                    MM(PS[bd][pr, o:o + 128], ktok[:, blk, h * 64:(h + 1) * 64], gvtok[:, blk, h * 128:(h + 1) * 128],
                       True, True, ["ktok"] + gvk, [pk(bd)])
            def state_step(blk):
                bd = 3 if blk < 2 else 7
                o = (blk % 2) * 256
                TTo("dve", Ust[:], Tst[:], PS[bd][:, o:o + 256], ALU.add, ["Tst", pk(bd)], ["Ust"])
                for P in range(2):
                    el = E1[:, P, blk * 128 + 127: blk * 128 + 128]
                    TS("dve", Tbf4[:, blk + 1, P * 128:(P + 1) * 128], Ust[:, P * 128:(P + 1) * 128], el, ALU.mult,
                       ["Ust", ("E1", P)], [("Tbf", blk + 1)])
                    TS("dve", Tst[:, P * 128:(P + 1) * 128], Ust[:, P * 128:(P + 1) * 128], el, ALU.mult,
                       ["Ust", ("E1", P)], ["Tst"])

            def s2_mm(blk):
                bc = slice(blk * 128, (blk + 1) * 128)
                am, amk = ams[blk]
                bo = OT_BANKS[blk]
                for h in range(4):
                    pr = slice(64 * (h % 2), 64 * (h % 2) + 64)
                    P = h // 2
                    MM(PS[bo][:, h * 128:(h + 1) * 128], gvtok[:, blk, h * 128:(h + 1) * 128], am[:, h * 128:(h + 1) * 128],
                       True, False, [amk] + gvk, [pk(bo)])
                    MM(PS[bo][:, h * 128:(h + 1) * 128], Tbf4[pr, blk, P * 128:(P + 1) * 128], gqT[pr, P, bc],
                       False, True, [("Tbf", blk), ("gqT", P)], [pk(bo)])
                sq, sqk = (sqb[:, blk, :], ("sqb", blk)) if blk < 2 else (sqe[:, blk - 2, :], ("sqe", blk - 2))
                ACT(sq, PS[bo][:], AF.Square, [pk(bo)], [sqk])
                return sq, sqk

            def s2_fin(blk, sqs):
                bc = slice(blk * 128, (blk + 1) * 128)
                sq, sqk = sqs
                bo = OT_BANKS[blk]
                bq = 7 if blk % 2 == 0 else 6
                MM(PS[bq][:], onesb, sq, True, True, [sqk, "cb"], [pk(bq)])
                rs, rsk = rF.next()
                ACT(rs, PS[bq][:], AF.Ln, [pk(bq)], [rsk], scale=1.0 / 128, bias=EPS)
                ACT(rs, rs, AF.Exp, [rsk], [rsk], scale=-0.5)
                og, ogk = rF.next()
                STT(og, PS[bo][:], col("gon"), rs, ALU.mult, ALU.mult, [pk(bo), rsk, "cols"], [ogk])
                TTo("dve", mixT[:, 4:8, bc], og.rearrange("p (h q) -> p h q", h=4), gsil[:, :, bc], ALU.mult,
                    [ogk] + [("gsil", h) for h in range(4)], [("mixG", blk)])

            sqs = []
            for blk in range(NBLK):
                state_step(blk)
                sqs.append(s2_mm(blk))
            for blk in range(NBLK - 1):
                s2_fin(blk, sqs[blk])
            wout_pass_a()
            s2_fin(NBLK - 1, sqs[NBLK - 1])
            A("pool", lambda e: e.tensor_copy(out=Tbf4[:, 0, :], in_=Tbf4[:, 4, :]), [("Tbf", 4)], [("Tbf", 0)])
            if mix_cut <= 7:
                return
            for i in range(1, 4):
                pw, pwk = W.acquire()
                pw3 = pw.rearrange("p (k c) -> p k c", k=8)
                if i == 1:
                    for dc in range(2):
                        pwa, pwak = wo_state["p"][dc // 2]
                        pwa3 = pwa.rearrange("p (k c) -> p k c", k=8)
                        b = WO_BANKS[dc]
                        dl = dc % 2
                        for kc in range(4, KC):
                            MM(PS[b][:], pwa3[:, kc, dl * 128:(dl + 1) * 128], mixT[:, kc, :], False, kc == KC - 1,
                               [pwak] + mixk, [pk(b)])
                        TTo("dve", hT[:, dc, :], PS[b][:], hT[:, dc, :], ALU.add, [pk(b), ("hT", dc)], [("hT", dc)])
                    W.release(1)
                for dl in range(2):
                    dc = 2 * i + dl
                    b = 4 + dl
                    for kc in range(KC):
                        MM(PS[b][:], pw3[:, kc, dl * 128:(dl + 1) * 128], mixT[:, kc, :], kc == 0, kc == KC - 1,
                           [pwk] + mixk, [pk(b)])
                    TTo("dve", hT[:, dc, :], PS[b][:], hT[:, dc, :], ALU.add, [pk(b), ("hT", dc)], [("hT", dc)])
                W.release()

        ple_state = {}

        def ple_emb_step(i):
            if i == 0:
                DMA("pool", ppbuf[:], w_d[n_pieces - 1], (), ["ppbuf"], lane="pp")
                ple_state["pp"] = (ppbuf[:], "ppbuf")
            if i in (0, 1):
                k2 = i
                b = 7
                for blk in range(NBLK):
                    TR(PS[b][:, blk * 128:(blk + 1) * 128], pin[:, blk, k2 * 128:(k2 + 1) * 128], ident, [("pin", blk), "cf"], [pk(b)])
                A("act", lambda e, b=b, k2=k2: e.copy(pT[:, k2, :], PS[b][:]), [pk(b)], [("pT", k2)])
                return
            pp, ppk = ple_state["pp"]
            pp3 = pp.rearrange("p (k c) -> p k c", k=2)
            if 2 <= i <= 9:
                dc = i - 2
                b = 7
                for k2 in range(2):
                    MM(PS[b][:], pp3[:, k2, dc * 128:(dc + 1) * 128], pT[:, k2, :], k2 == 0, k2 == 1, [ppk, ("pT", k2)], [pk(b)])
                A("act", lambda e, b=b, dc=dc: e.copy(embF[:, dc, :], PS[b][:]), [pk(b)], [("embF", dc)])
                sq, sqk = sqe[:, dc % 2, :], ("sqe", dc % 2)
                ACT(sq, PS[b][:], AF.Square, [pk(b)], [sqk])
                ple_state["sq%d" % dc] = (sq, sqk)
            if 3 <= i <= 10:
                dc = i - 3
                sq, sqk = ple_state["sq%d" % dc]
                MM(PS[6][:], onesb, sq, dc == 0, dc == KC - 1, [sqk, "cb"], [pk(6)])
            if i == 10:
                rs, rsk = c16[:, 0, :], ("c16", 0)
                ACT(rs, PS[6][:], AF.Ln, [pk(6)], [rsk], scale=1.0 / D, bias=EPS)
                ACT(rs, rs, AF.Exp, [rsk], [rsk], scale=-0.5)
                for dc in range(KC):
                    STT(embF[:, dc, :], embF[:, dc, :], col("npl", dc), rs, ALU.mult, ALU.mult,
                        [("embF", dc), rsk, "cols"], [("embF", dc)])

        def ple(t):
            norm_to_nT("npg")
            ems = [(embF[:, dc, :], ("embF", dc)) for dc in range(KC)]
            for i4 in range(4):
                pg, pgk2 = W.acquire()
                pg3 = pg.rearrange("p (k c) -> p k c", k=8)
                if i4 == 0:
                    for kc in range(KC):
                        for dl in range(2):
                            MM(PS[4 + dl][:], pg3[:, kc, dl * 128:(dl + 1) * 128], nT[:, kc, :], kc == 0, kc == KC - 1,
                               [pgk2, ("nT", kc)], [pk(4 + dl)])
                for dl in range(2):
                    dc = 2 * i4 + dl
                    b = 4 + dl
                    for kc in range(KC if i4 > 0 else 0):
                        MM(PS[b][:], pg3[:, kc, dl * 128:(dl + 1) * 128], nT[:, kc, :], kc == 0, kc == KC - 1,
                           [pgk2, ("nT", kc)], [pk(b)])
                    gt, gtk = rF.next()
                    ACT(gt, PS[b][:], AF.Sigmoid, [pk(b)], [gtk])
                    em, emk = ems[dc]
                    TTo("dve", em, em, gt, ALU.mult, [emk, gtk], [emk])
                    TTo("dve", hT[:, dc, :], hT[:, dc, :], em, ALU.add, [emk, ("hT", dc)], [("hT", dc)])
                W.release()

        for t in range(n_tiles):
            load_tile(t)
            if stop_after != "load":
                ffn("n1")
            if stop_after not in ("load", "ffn1"):
                mixer(t)
            if stop_after not in ("load", "ffn1", "mix"):
                ffn("n2", extra=ple_emb_step if stop_after is None else None)
            if stop_after not in ("load", "ffn1", "mix", "ffn2"):
                ple(t)
            store_tile(t)
        S.emit(final_waits=out_ops[-4:])
    return nc


_CACHE = {}


def kernel(**inputs):
    packed = _host_pack(inputs)
    x = np.asarray(inputs["x"], np.float32)
    p = np.asarray(inputs["p"], np.float32)[0]
    B = x.shape[0]
    if "nc" not in _CACHE:
        _CACHE["nc"] = build_nc()
    nc = _CACHE["nc"]
    in_maps = []
    for b in range(B):
        m = dict(packed)
        m["x"] = np.ascontiguousarray(x[b])
        m["p"] = np.ascontiguousarray(p[b])
        in_maps.append(m)
    res = run_bass_kernel_spmd(nc, in_maps, core_ids=list(range(B)))
    return np.stack([np.asarray(r["out"], np.float32) for r in res.results], 0)
```

```python
from contextlib import ExitStack
import numpy as np
import concourse.bass as bass
import concourse.mybir as mybir
from concourse.bass_utils import run_bass_kernel_spmd

F32 = mybir.dt.float32
BF16 = mybir.dt.bfloat16
AF = mybir.ActivationFunctionType
ALU = mybir.AluOpType

S_LEN = 4096
D = 1024
KC = 8
FF = 2816
FC = 22
TT = 512
NBLK = 4
EPS = 1e-6
RING = 11
PIECE = 2048


class Sched:
    def __init__(self, nc):
        self.nc = nc
        self.ops = []
        self.lw = {}
        self.rd = {}
        self.lane_last = {}
        self.lane_cnt = {}

    def add(self, eng, fn, reads=(), writes=(), lane=None):
        i = len(self.ops)
        me = ("lane", lane) if lane is not None else eng
        raw, other = set(), set()
        for k in reads:
            raw.update(self.lw.get(k, {}).values())
        for k in writes:
            other.update(self.lw.get(k, {}).values())
            other.update(self.rd.get(k, {}).values())
        if lane is not None and lane in self.lane_last:
            raw.add(self.lane_last[lane])
        deps = set()
        for d in raw | other:
            y = self.ops[d]
            if y["lane"] is None and lane is None and y["eng"] == eng:
                if eng == "pe" or d not in raw:
                    continue
            deps.add(d)
        for k in reads:
            self.rd.setdefault(k, {})[me] = i
        for k in writes:
            self.lw.setdefault(k, {})[me] = i
            self.rd[k] = {}
        op = dict(eng=eng, fn=fn, deps=deps, lane=lane, sig=False, val=None)
        if lane is not None:
            self.lane_last[lane] = i
            self.lane_cnt[lane] = self.lane_cnt.get(lane, 0) + 16
            op["val"] = self.lane_cnt[lane]
        self.ops.append(op)
        return i

    def emit(self, final_waits=()):
        nc = self.nc
        ops = self.ops
        for op in ops:
            for d in op["deps"]:
                ops[d]["sig"] = True
        for d in final_waits:
            ops[d]["sig"] = True
        cnt = {}
        for op in ops:
            if op["lane"] is None and op["sig"]:
                cnt[op["eng"]] = cnt.get(op["eng"], 0) + 1
                op["val"] = cnt[op["eng"]]
        engs = ["pe", "act", "dve", "pool", "sp"]
        with ExitStack() as st:
            esem = {e: st.enter_context(nc.semaphore("s_" + e)) for e in engs}
            lsem = {l: st.enter_context(nc.semaphore("l_%s" % (l,))) for l in self.lane_cnt}
            block = st.enter_context(nc.Block())

            def semof(op):
                return lsem[op["lane"]] if op["lane"] is not None else esem[op["eng"]]

            def run(ename):
                def body(eng):
                    waited = {}
                    for op in ops:
                        if op["eng"] != ename:
                            continue
                        need = {}
                        for d in op["deps"]:
                            y = ops[d]
                            s = semof(y)
                            if y["val"] > need.get(id(s), (None, 0))[1]:
                                need[id(s)] = (s, y["val"])
                        for sid, (s, v) in need.items():
                            if waited.get(sid, 0) < v:
                                eng.wait_ge(s, v)
                                waited[sid] = v
                        ins = op["fn"](eng)
                        if op["lane"] is not None:
                            ins.then_inc(lsem[op["lane"]], 16)
                        elif op["sig"]:
                            ins.then_inc(esem[ename], 1)
                    if ename == "sp":
                        for d in final_waits:
                            y = ops[d]
                            eng.wait_ge(semof(y), y["val"])
                return body

            block.tensor(run("pe"))
            block.scalar(run("act"))
            block.vector(run("dve"))
            block.gpsimd(run("pool"))
            block.sync(run("sp"))


COLS = dict(n1=0, nm=8, n2=16, npg=24, npl=32, qn=40, kn=41, gon=42, bg=43, sink=45)
NCOLS = 49
CF = dict(ident=0, glamask=128, scanmask=640)
NCF = 1152
CB = dict(ident=0, pswap=128, bones=256, ones=384, mdiag=512, mprev=1024)
NCB = 1536

Q_PERM = [0, 4, 1, 5, 2, 6, 3, 7]


def _fmtA(W, cols):
    K = W.shape[0]
    kc = K // 128
    out = np.zeros((128, 8, 256), np.float32)
    out[:, :kc, : len(cols)] = W[:, cols].reshape(kc, 128, len(cols)).transpose(1, 0, 2)
    return out.reshape(128, PIECE)


def _down_pieces(Wd):
    blocks = Wd.reshape(FC, 128, KC, 128).transpose(2, 0, 1, 3).reshape(KC * FC, 128, 128)
    pcs = blocks.reshape(11, 16, 128, 128).transpose(0, 2, 1, 3).reshape(11, 128, PIECE)
    return [pcs[i] for i in range(11)]


def _ffn_pieces(wg, wu, wd):
    out = []
    for i in range(11):
        cols = np.arange(256 * i, 256 * i + 256)
        out.append(_fmtA(wg, cols))
        out.append(_fmtA(wu, cols))
    out += _down_pieces(wd)
    return out


def _host_pack(inp):
    g = lambda k: np.asarray(inp[k], np.float32)[0]
    pieces = []
    pieces += _ffn_pieces(g("ffn1_w_gate"), g("ffn1_w_up"), g("ffn1_w_down"))
    w_in = g("w_in")
    qcols = np.concatenate([np.arange(h * 64, h * 64 + 64) for h in Q_PERM])
    pieces.append(_fmtA(w_in, qcols[:256]))
    pieces.append(_fmtA(w_in, qcols[256:]))
    pieces.append(_fmtA(w_in, np.arange(512, 768)))
    pieces.append(_fmtA(w_in, np.arange(1792, 1808)))
    pieces.append(_fmtA(w_in, np.arange(768, 1024)))
    pieces.append(_fmtA(w_in, np.arange(1024, 1280)))
    pieces.append(_fmtA(w_in, np.arange(1808, 2064)))
    pieces.append(_fmtA(w_in, np.arange(2064, 2320)))
    pieces.append(_fmtA(w_in, np.arange(1280, 1536)))
    pieces.append(_fmtA(w_in, np.arange(1536, 1792)))
    w_out = g("w_out")
    rows = np.concatenate([np.arange(h * 64, h * 64 + 64) for h in Q_PERM] + [np.arange(512, 1024)])
    w_out_p = w_out[rows]
    for i in range(4):
        pieces.append(_fmtA(w_out_p, np.arange(256 * i, 256 * i + 256)))
    pieces += _ffn_pieces(g("ffn2_w_gate"), g("ffn2_w_up"), g("ffn2_w_down"))
    wpg = g("ple_w_gate")
    for i in range(4):
        pieces.append(_fmtA(wpg, np.arange(256 * i, 256 * i + 256)))
    wpp = g("ple_w_proj")
    pieces.append(wpp.reshape(2, 128, 1024).transpose(1, 0, 2).reshape(128, PIECE))
    wstream = np.ascontiguousarray(np.stack(pieces, 0))

    cols = np.zeros((128, NCOLS), np.float32)
    for name, key in (("n1", "ffn1_norm"), ("nm", "mix_norm"), ("n2", "ffn2_norm"), ("npg", "ple_gate_norm"), ("npl", "ple_norm")):
        cols[:, COLS[name]: COLS[name] + 8] = g(key).reshape(8, 128).T
    cols[:, COLS["qn"]] = np.tile(g("q_norm"), 2)
    cols[:, COLS["kn"]] = np.tile(g("k_norm"), 2)
    cols[:, COLS["gon"]] = g("gla_out_norm")
    cols[:, COLS["bg"]: COLS["bg"] + 2] = g("gla_b_gate").reshape(2, 128).T
    sk = g("attn_sinks")
    for c in range(4):
        cols[:64, COLS["sink"] + c] = sk[c]
        cols[64:, COLS["sink"] + c] = sk[c + 4]

    cf = np.zeros((128, NCF), np.float32)
    cf[:, 0:128] = np.eye(128, dtype=np.float32)
    j = np.arange(128)[:, None]
    i = np.arange(128)[None, :]
    cf[:, 128:640] = np.tile((j <= i).astype(np.float32), (1, 4))
    sm = np.ones((128, 512), np.float32)
    sm[:, ::128] = 0.0
    cf[:, 640:1152] = sm

    cb = np.zeros((128, NCB), np.float32)
    cb[:, 0:128] = np.eye(128, dtype=np.float32)
    m = np.arange(128)
    swp = (m // 64) * 64 + ((m % 64) + 32) % 64
    cb[swp, 128 + m] = 1.0
    cb[:, 256:384] = ((j // 64) == (i // 64)).astype(np.float32)
    cb[:, 384:512] = 1.0
    NEG = -30000.0
    cb[:, 512:1024] = np.tile(np.where(j <= i, 0.0, NEG).astype(np.float32), (1, 4))
    cb[:, 1024:1536] = np.tile(np.where(j > i, 0.0, NEG).astype(np.float32), (1, 4))

    pos = np.arange(S_LEN).astype(np.float64)
    inv_freq = 10000.0 ** (-np.arange(0, 64, 2, dtype=np.float64) / 64)
    ang = pos[:, None] * inv_freq[None, :]
    cs = np.cos(ang).astype(np.float32).T
    sn = np.sin(ang).astype(np.float32).T
    cos2 = np.ascontiguousarray(np.tile(cs, (4, 1)))
    sin2s = np.ascontiguousarray(np.tile(np.concatenate([-sn, sn], 0), (2, 1)))
    return dict(wstream=wstream, cols=cols, cf=cf, cb=cb, cos2=cos2, sin2s=sin2s, wgu=g("gla_w_gate_up"))


def build_nc(n_tiles=S_LEN // TT, n_pieces=85, stop_after=None, mix_cut=99):
    nc = bass.Bass("TRN2", target_bir_lowering=False)
    dr = lambda n, s, k="ExternalInput": nc.dram_tensor(n, s, F32, kind=k).ap()
    x_d = dr("x", [S_LEN, D])
    p_d = dr("p", [S_LEN, 256])
    w_d = dr("wstream", [n_pieces, 128, PIECE])
    cols_d = dr("cols", [128, NCOLS])
    cf_d = dr("cf", [128, NCF])
    cb_d = dr("cb", [128, NCB])
    cos_d = dr("cos2", [128, S_LEN])
    sin_d = dr("sin2s", [128, S_LEN])
    wgu_d = dr("wgu", [16, 256])
    out_d = dr("out", [S_LEN, D], "ExternalOutput")

    with ExitStack() as st:
        sb = lambda n, s, d=F32: st.enter_context(nc.sbuf_tensor(n, s, d))
        S = Sched(nc)
        A = S.add

        PS = [st.enter_context(nc.psum_tensor("ps%d" % i, [128, 512], F32)) for i in range(8)]
        pk = lambda b: ("ps", b)
        hT = sb("hT", [128, KC, TT])
        nT = sb("nT", [128, KC, TT], BF16)
        actT = sb("actT", [128, FC, TT], BF16)
        ring = sb("ring", [128, RING, PIECE], BF16)
        xin = sb("xin", [128, 2, D])
        osb = sb("osb", [128, 2, D])
        pin = sb("pin", [128, NBLK, 256])
        pT = sb("pT", [128, 2, TT], BF16)
        sqb = sb("sqb", [128, 2, TT], BF16)
        sqe = sb("sqe", [128, 2, TT], BF16)
        tmpF = sb("tmpF", [128, 6, TT])
        tmpB = sb("tmpB", [128, 4, TT], BF16)
        qT = sb("qT", [128, 4, TT], BF16)
        kTa = sb("kTa", [128, 5 * 128], BF16)
        vtok = sb("vtok", [128, 5, 128], BF16)
        gvtok = sb("gvtok", [128, NBLK, 512], BF16)
        gqT = sb("gqT", [128, 2, TT], BF16)
        gkT = sb("gkT", [128, 2, TT], BF16)
        ktok = sb("ktok", [128, NBLK, 256], BF16)
        c16 = sb("c16", [128, 2, TT])
        E1 = sb("E1", [128, 2, TT])
        gsil = sb("gsil", [128, 4, TT], BF16)
        glrT = sb("glrT", [16, TT], BF16)
        mixT = sb("mixT", [128, KC, TT], BF16)
        Tst = sb("Tst", [128, 256])
        Ust = sb("Ust", [128, 256])
        Tbf4 = sb("Tbf4", [128, 5, 256], BF16)
        embF = sb("embF", [128, KC, TT])
        ppbuf = sb("ppbuf", [128, PIECE], BF16)
        cosT = sb("cosT", [128, TT])
        sinT = sb("sinT", [128, TT])
        cols = sb("colsb", [128, NCOLS])
        negb = sb("negb", [128, 2])
        sinke = sb("sinke", [128, 4])
        cf = sb("cfb", [128, NCF])
        cb = sb("cbb", [128, NCB], BF16)
        wgu = sb("wgub", [16, 256], BF16)

        ident = cf[:, 0:128]
        glamask = cf[:, 128:640]
        scanmask = cf[:, 640:1152]
        identb = cb[:, 0:128]
        pswap = cb[:, 128:256]
        bones = cb[:, 256:384]
        onesb = cb[:, 384:512]
        mdiag = cb[:, 512:1024]
        mprev = cb[:, 1024:1536]
        col = lambda name, i=0: cols[:, COLS[name] + i: COLS[name] + i + 1]

        class Rot:
            def __init__(self, t, n, name):
                self.t, self.n, self.name, self.i = t, n, name, 0

            def next(self):
                j = self.i % self.n
                self.i += 1
                return self.t[:, j, :], (self.name, j)

        rF = Rot(tmpF, 6, "tmpF")
        rB = Rot(tmpB, 4, "tmpB")
        rSq = Rot(sqb, 2, "sqb")

        def MM(out, lhsT, rhs, st_, sp_, r, w):
            A("pe", lambda e: e.matmul(out, lhsT, rhs, start=st_, stop=sp_), r, w)

        def TR(out, in_, idt, r, w):
            A("pe", lambda e: e.transpose(out, in_, idt), r, w)

        def ACT(out, in_, func, r, w, scale=None, bias=None):
            kw = {}
            if scale is not None:
                kw["scale"] = scale
            if bias is not None:
                kw["bias"] = bias
            A("act", lambda e: e.activation(out=out, in_=in_, func=func, **kw), r, w)

        def TTo(eng, out, in0, in1, op, r, w):
            A(eng, lambda e: e.tensor_tensor(out=out, in0=in0, in1=in1, op=op), r, w)

        def STT(out, in0, scalar, in1, op0, op1, r, w):
            A("dve", lambda e: e.scalar_tensor_tensor(out=out, in0=in0, scalar=scalar, in1=in1, op0=op0, op1=op1), r, w)

        def TS(eng, out, in0, s1, op0, r, w):
            A(eng, lambda e: e.tensor_scalar(out=out, in0=in0, scalar1=s1, scalar2=None, op0=op0), r, w)

        def DMA(q, out, in_, r, w, lane):
            return A(q, lambda e: e.dma_start(out=out, in_=in_), r, w, lane=lane)

        class WStream:
            def __init__(self):
                self.issued = 0
                self.acq = 0
                self.released = 0
                self.total = n_tiles * (n_pieces - 1)
                self.done = {}
                self.pinned = set()

            def pump(self):
                while self.done.get(self.released, False):
                    self.released += 1
                while self.issued < min(self.total, self.released + RING):
                    g = self.issued
                    slot = g % RING
                    pc = g % (n_pieces - 1)
                    DMA("pool", ring[:, slot, :], w_d[pc], (), [("ring", slot)], lane=("w", slot))
                    self.issued += 1

            def acquire(self, pin=False):
                g = self.acq
                self.acq += 1
                self.pump()
                assert g < self.issued, (g, self.issued, self.released)
                if pin:
                    self.pinned.add(g)
                self.last = g
                slot = g % RING
                return ring[:, slot, :], ("ring", slot)

            def release(self, n=1):
                g = self.released
                while n > 0:
                    assert g < self.acq
                    if not self.done.get(g, False) and g not in self.pinned:
                        self.done[g] = True
                        n -= 1
                    g += 1
                self.pump()

            def unpin(self, g):
                self.pinned.discard(g)
                self.done[g] = True
                self.pump()

        W = WStream()

        DMA("sp", cols[:], cols_d, (), ["cols"], "c0")
        DMA("sp", cf[:], cf_d, (), ["cf"], "c1")
        DMA("pool", cb[:], cb_d, (), ["cb"], "c2")
        DMA("pool", wgu[:], wgu_d, (), ["wgu"], "c3")
        TS("dve", negb[:], cols[:, COLS["bg"]: COLS["bg"] + 2], -1.0, ALU.mult, ["cols"], ["negb"])
        ACT(sinke[:], cols[:, COLS["sink"]: COLS["sink"] + 4], AF.Exp, ["cols"], ["sinke"])
        A("dve", lambda e: e.memset(Tst[:], 0.0), (), ["Tst"])
        A("dve", lambda e: e.memset(Tbf4[:, 0, :], 0.0), (), [("Tbf", 0)])

        def rms_feature_major(src_keys, src_aps, gname, nchunks, dim):
            for kc in range(nchunks):
                sq, sqk = rSq.next()
                if gname == "n1":
                    TTo("dve", sq, src_aps[kc], src_aps[kc], ALU.mult, [src_keys[kc]], [sqk])
                else:
                    ACT(sq, src_aps[kc], AF.Square, [src_keys[kc]], [sqk])
                MM(PS[6][:], onesb, sq, kc == 0, kc == nchunks - 1, [sqk, "cb"], [pk(6)])
            t, tk = rF.next()
            ACT(t, PS[6][:], AF.Ln, [pk(6)], [tk], scale=1.0 / dim, bias=EPS)
            ACT(t, t, AF.Exp, [tk], [tk], scale=-0.5)
            return t, tk

        def norm_to_nT(gname):
            rstd, rk = rms_feature_major([("hT", kc) for kc in range(KC)], [hT[:, kc, :] for kc in range(KC)], gname, KC, D)
            for kc in range(KC):
                STT(nT[:, kc, :], hT[:, kc, :], col(gname, kc), rstd, ALU.mult, ALU.mult,
                    [("hT", kc), rk, "cols"], [("nT", kc)])

        def ffn(gname, extra=None):
            norm_to_nT(gname)
            for i in range(11):
                pg, pgk = W.acquire()
                pu, puk = W.acquire()
                pg3 = pg.rearrange("p (k c) -> p k c", k=8)
                pu3 = pu.rearrange("p (k c) -> p k c", k=8)
                if i == 0:
                    for kc in range(KC):
                        for fl in range(2):
                            bg_, bu_ = (0, 1) if fl == 0 else (2, 3)
                            MM(PS[bg_][:], pg3[:, kc, fl * 128:(fl + 1) * 128], nT[:, kc, :], kc == 0, kc == KC - 1,
                               [pgk, ("nT", kc)], [pk(bg_)])
                            MM(PS[bu_][:], pu3[:, kc, fl * 128:(fl + 1) * 128], nT[:, kc, :], kc == 0, kc == KC - 1,
                               [puk, ("nT", kc)], [pk(bu_)])
                for fl in range(2):
                    fc = 2 * i + fl
                    bg_, bu_ = (0, 1) if fc % 2 == 0 else (2, 3)
                    for kc in range(KC if i > 0 else 0):
                        MM(PS[bg_][:], pg3[:, kc, fl * 128:(fl + 1) * 128], nT[:, kc, :], kc == 0, kc == KC - 1,
                           [pgk, ("nT", kc)], [pk(bg_)])
                    for kc in range(KC if i > 0 else 0):
                        MM(PS[bu_][:], pu3[:, kc, fl * 128:(fl + 1) * 128], nT[:, kc, :], kc == 0, kc == KC - 1,
                           [puk, ("nT", kc)], [pk(bu_)])
                    sg, sgk = rF.next()
                    ACT(sg, PS[bg_][:], AF.Silu, [pk(bg_)], [sgk])
                    TTo("dve", actT[:, fc, :], sg, PS[bu_][:], ALU.mult, [sgk, pk(bu_)], [("act", fc)])
                W.release(2)
                if extra is not None:
                    extra(i)
            cur = None
            for dc in range(KC):
                b = 4 + dc % 2
                for fc in range(FC):
                    bi = dc * FC + fc
                    if bi % 16 == 0:
                        if cur is not None:
                            W.release()
                        cur = W.acquire()
                    pd, pdk = cur
                    o = (bi % 16) * 128
                    MM(PS[b][:], pd[:, o:o + 128], actT[:, fc, :], fc == 0, fc == FC - 1, [pdk, ("act", fc)], [pk(b)])
                STT(hT[:, dc, :], PS[b][:], 0.5, hT[:, dc, :], ALU.mult, ALU.add, [pk(b), ("hT", dc)], [("hT", dc)])
            W.release()

        def issue_x(t, blk):
            r0 = t * TT + blk * 128
            xb = blk % 2
            DMA("sp", xin[:, xb, :], x_d[r0:r0 + 128, :], (), [("xin", xb)], ("xin", xb))

        def load_tile(t):
            if t == 0:
                issue_x(0, 0)
                issue_x(0, 1)
            DMA("sp", cosT[:], cos_d[:, t * TT:(t + 1) * TT], (), ["cosT"], "cos")
            DMA("sp", sinT[:], sin_d[:, t * TT:(t + 1) * TT], (), ["sinT"], "sin")
            for blk in range(NBLK):
                xb = blk % 2
                for half in range(2):
                    b = (7, 6, 2, 3)[(blk % 2) * 2 + half]
                    for kl in range(4):
                        kc = half * 4 + kl
                        TR(PS[b][:, kl * 128:(kl + 1) * 128], xin[:, xb, kc * 128:(kc + 1) * 128], ident,
                           [("xin", xb), "cf"], [pk(b)])
                    A("act", lambda e, b=b, half=half, blk=blk: e.copy(
                        hT[:, half * 4:half * 4 + 4, blk * 128:(blk + 1) * 128],
                        PS[b][:].rearrange("p (k c) -> p k c", k=4)),
                      [pk(b)], [("hT", half * 4 + kl) for kl in range(4)])
                if blk < 2:
                    issue_x(t, blk + 2)
                elif t + 1 < n_tiles:
                    issue_x(t + 1, blk - 2)
            for blk in range(NBLK):
                r0 = t * TT + blk * 128
                DMA("sp", pin[:, blk, :], p_d[r0:r0 + 128, :], (), [("pin", blk)], ("pin", blk))

        out_ops = []

        def store_tile(t):
            mix32 = mixT[:].rearrange("p k t -> p (k t)").bitcast(F32).rearrange("p (s d) -> p s d", s=2)
            for blk in range(NBLK):
                r0 = t * TT + blk * 128
                if blk < 2:
                    stage = osb[:, blk, :]
                    keys = [[("osb", blk, 0)], [("osb", blk, 1)]]
                else:
                    stage = mix32[:, blk - 2, :]
                    kk = [("mixA" if blk == 2 else "mixG", b_) for b_ in range(NBLK)]
                    keys = [kk, kk]
                for half in range(2):
                    b = (7, 6, 2, 3)[(blk % 2) * 2 + half]
                    for kl in range(4):
                        kc = half * 4 + kl
                        TR(PS[b][:, kl * 128:(kl + 1) * 128], hT[:, kc, blk * 128:(blk + 1) * 128], ident,
                           [("hT", kc), "cf"], [pk(b)])
                    A("act", lambda e, b=b, half=half, stage=stage: e.copy(stage[:, half * 512:(half + 1) * 512], PS[b][:]),
                      [pk(b)], keys[half])
                out_ops.append(DMA("sp", out_d[r0:r0 + 128, :], stage, keys[0] + keys[1], (), ("out", blk)))

        def qk_A(piece3, pkey, cl, gname, bset, skip_mm=False):
            bx = bset[0]
            for kc in range(0 if skip_mm else KC):
                MM(PS[bx][:], piece3[:, kc, cl * 128:(cl + 1) * 128], nT[:, kc, :], kc == 0, kc == KC - 1,
                   [pkey, ("nT", kc)], [pk(bx)])
            sq, sqk = rSq.next()
            ACT(sq, PS[bx][:], AF.Square, [pk(bx)], [sqk])
            xg, xgk = rB.next()
            ACT(xg, PS[bx][:], AF.Identity, [pk(bx), "cols"], [xgk], scale=col(gname))
            return sq, sqk, xg, xgk

        def qk_B(stt, bset, dst, dstk):
            sq, sqk, xg, xgk = stt
            bs, bw = bset[1], bset[2]
            MM(PS[bs][:], bones, sq, True, True, [sqk, "cb"], [pk(bs)])
            MM(PS[bw][:], pswap, xg, True, True, [xgk, "cb"], [pk(bw)])
            rs, rsk = rF.next()
            ACT(rs, PS[bs][:], AF.Ln, [pk(bs)], [rsk], scale=1.0 / 64, bias=EPS)
            ACT(rs, rs, AF.Exp, [rsk], [rsk], scale=-0.5)
            t1, t1k = rF.next()
            TTo("pool", t1, xg, cosT[:], ALU.mult, [xgk, "cosT"], [t1k])
            t2, t2k = rF.next()
            TTo("dve", t2, PS[bw][:], sinT[:], ALU.mult, [pk(bw), "sinT"], [t2k])
            TTo("dve", t2, t2, t1, ALU.add, [t2k, t1k], [t2k])
            TTo("dve", dst, t2, rs, ALU.mult, [t2k, rsk], [dstk])

        def mixer(t):
            norm_to_nT("nm")
            bsets = [(0, 1, 2), (3, 4, 5)]
            pq0, pq0k = W.acquire()
            pq03 = pq0.rearrange("p (k c) -> p k c", k=8)
            for kc in range(KC):
                for cl in range(2):
                    MM(PS[bsets[cl][0]][:], pq03[:, kc, cl * 128:(cl + 1) * 128], nT[:, kc, :], kc == 0, kc == KC - 1,
                       [pq0k, ("nT", kc)], [pk(bsets[cl][0])])
            st0 = qk_A(pq03, pq0k, 0, "qn", bsets[0], skip_mm=True)
            st1 = qk_A(pq03, pq0k, 1, "qn", bsets[1], skip_mm=True)
            W.release()
            qk_B(st0, bsets[0], qT[:, 0, :], ("qT", 0))
            pq1, pq1k = W.acquire()
            pq13 = pq1.rearrange("p (k c) -> p k c", k=8)
            st2 = qk_A(pq13, pq1k, 0, "qn", bsets[0])
            qk_B(st1, bsets[1], qT[:, 1, :], ("qT", 1))
            st3 = qk_A(pq13, pq1k, 1, "qn", bsets[1])
            W.release()
            qk_B(st2, bsets[0], qT[:, 2, :], ("qT", 2))
            pa, pak = W.acquire()
            pa3 = pa.rearrange("p (k c) -> p k c", k=8)
            st4 = qk_A(pa3, pak, 0, "kn", bsets[0])
            qk_B(st3, bsets[1], qT[:, 3, :], ("qT", 3))
            for blk in range(NBLK):
                for kc in range(KC):
                    MM(PS[6][:, blk * 128:(blk + 1) * 128], nT[:, kc, blk * 128:(blk + 1) * 128], pa3[:, kc, 128:256],
                       kc == 0, kc == KC - 1, [pak, ("nT", kc)], [pk(6)])
            A("act", lambda e: e.copy(vtok[:, 1:5, :], PS[6][:].rearrange("p (b c) -> p b c", b=4)), [pk(6)], ["vtok"])
            W.release()
            qk_B(st4, bsets[0], kTa[:, 128:640], "kT")
            if mix_cut <= 2:
                return
            pl, plk = W.acquire()
            pl3 = pl.rearrange("p (k c) -> p k c", k=8)
            for kc in range(KC):
                MM(PS[7][0:16, :], pl3[:, kc, 0:16], nT[:, kc, :], kc == 0, kc == KC - 1, [plk, ("nT", kc)], [pk(7)])
            A("act", lambda e: e.copy(glrT[:], PS[7][0:16, :]), [pk(7)], ["glrT"])
            W.release()
            pgq, pgqk = W.acquire()
            pgk_, pgkk = W.acquire()
            pgq3 = pgq.rearrange("p (k c) -> p k c", k=8)
            pgk3 = pgk_.rearrange("p (k c) -> p k c", k=8)
            for P in range(2):
                MM(PS[6][:], wgu[:, P * 128:(P + 1) * 128], glrT[:], True, True, ["wgu", "glrT"], [pk(6)])
                sp_, spk = rF.next()
                ACT(sp_, PS[6][:], AF.Exp, [pk(6), "negb"], [spk], scale=-1.0, bias=negb[:, P:P + 1])
                ACT(sp_, sp_, AF.Ln, [spk], [spk], bias=1.0)
                A("dve", lambda e, P=P, sp_=sp_: e.tensor_tensor_scan(out=c16[:, P, :], data0=scanmask, data1=sp_, initial=0.0,
                                                                    op0=ALU.mult, op1=ALU.add),
                  [spk, "cf"], [("c16", P)])
                ACT(E1[:, P, :], c16[:, P, :], AF.Exp, [("c16", P)], [("E1", P)], scale=-1.0 / 16)
                e2, e2k = rF.next()
                ACT(e2, c16[:, P, :], AF.Exp, [("c16", P)], [e2k], scale=1.0 / 16)
                for kc in range(KC):
                    MM(PS[0][:], pgq3[:, kc, P * 128:(P + 1) * 128], nT[:, kc, :], kc == 0, kc == KC - 1,
                       [pgqk, ("nT", kc)], [pk(0)])
                STT(gqT[:, P, :], PS[0][:], 0.125, E1[:, P, :], ALU.mult, ALU.mult, [pk(0), ("E1", P)], [("gqT", P)])
                for kc in range(KC):
                    MM(PS[1][:], pgk3[:, kc, P * 128:(P + 1) * 128], nT[:, kc, :], kc == 0, kc == KC - 1,
                       [pgkk, ("nT", kc)], [pk(1)])
                TTo("dve", gkT[:, P, :], PS[1][:], e2, ALU.mult, [pk(1), e2k], [("gkT", P)])
            W.release(2)
            ps7b = PS[7][:].bitcast(BF16)
            for blk in range(NBLK):
                for P in range(2):
                    o = blk * 256 + P * 128
                    TR(ps7b[:, o:o + 128], gkT[:, P, blk * 128:(blk + 1) * 128], identb, [("gkT", P), "cb"], [pk(7)])
            A("act", lambda e: e.copy(ktok[:].rearrange("p b c -> p (b c)"), ps7b), [pk(7)], ["ktok"])
            for hg in range(2):
                po, pok = W.acquire()
                po3 = po.rearrange("p (k c) -> p k c", k=8)
                for hl in range(2):
                    h = hg * 2 + hl
                    b = 4 + hl
                    for kc in range(KC):
                        MM(PS[b][:], po3[:, kc, hl * 128:(hl + 1) * 128], nT[:, kc, :], kc == 0, kc == KC - 1,
                           [pok, ("nT", kc)], [pk(b)])
                    ACT(gsil[:, h, :], PS[b][:], AF.Silu, [pk(b)], [("gsil", h)])
                W.release()
            gv_state = {}

            def gv_unit(u):
                hv, bp = u // 2, u % 2
                if bp == 0:
                    gv_state["p"] = W.acquire()
                pv, pvk = gv_state["p"]
                pv3 = pv.rearrange("p (k c) -> p k c", k=8)
                b = 2 + bp
                for bl in range(2):
                    blk_ = bp * 2 + bl
                    for kc in range(KC):
                        MM(PS[b][:, bl * 256:(bl + 1) * 256], nT[:, kc, blk_ * 128:(blk_ + 1) * 128], pv3[:, kc, :],
                           kc == 0, kc == KC - 1, [pvk, ("nT", kc)], [pk(b)])
                A("act", lambda e, b=b, bp=bp, hv=hv: e.copy(
                    gvtok[:, bp * 2:bp * 2 + 2, hv * 256:(hv + 1) * 256],
                    PS[b][:].rearrange("p (b c) -> p b c", b=2)), [pk(b)], [("gvtok", hv, bp)])
                if bp == 1:
                    W.release()

            for blk in range(NBLK):
                first = (t == 0 and blk == 0)
                seq = ["cur"] if first else ["prev", "cur"]
                es = {}
                for j in range(2):
                    pr = slice(64 * j, 64 * j + 64)
                    for which in seq:
                        kb = blk if which == "prev" else blk + 1
                        bS = 4 + 2 * j + (0 if which == "prev" else 1)
                        MM(PS[bS][:], kTa[pr, kb * 128:(kb + 1) * 128], qT[pr, :, blk * 128:(blk + 1) * 128], True, False,
                           ["kT", "kTprev"] + [("qT", c) for c in range(4)], [pk(bS)])
                        MM(PS[bS][:], identb, mprev if which == "prev" else mdiag, False, True, ["cb"], [pk(bS)])
                        e_, ek = rB.next()
                        ACT(e_, PS[bS][:], AF.Exp, [pk(bS)], [ek], scale=0.125)
                        es[(j, which)] = (e_, ek)
                gv_unit(blk)
                for j in range(2):
                    pr = slice(64 * j, 64 * j + 64)
                    for n_, which in enumerate(seq):
                        vb = blk if which == "prev" else blk + 1
                        e_, ek = es[(j, which)]
                        MM(PS[0][pr, :], vtok[:, vb, 64 * j:64 * j + 64], e_, n_ == 0, n_ == len(seq) - 1,
                           [ek, "vtok", "vprev"], [pk(0)])
                    for n_, which in enumerate(seq):
                        e_, ek = es[(j, which)]
                        MM(PS[1][pr, :], onesb[:, 0:64], e_, n_ == 0, n_ == len(seq) - 1, [ek, "cb"], [pk(1)])
                ld, ldk = rF.next()
                for c in range(4):
                    ACT(ld[:, c * 128:(c + 1) * 128], PS[1][:, c * 128:(c + 1) * 128], AF.Ln, [pk(1), "sinke"], [ldk],
                        bias=sinke[:, c:c + 1])
                ACT(ld, ld, AF.Exp, [ldk], [ldk], scale=-1.0)
                TTo("dve", mixT[:, 0:4, blk * 128:(blk + 1) * 128], PS[0][:].rearrange("p (c q) -> p c q", c=4),
                    ld.rearrange("p (c q) -> p c q", c=4), ALU.mult, [pk(0), ldk], [("mixA", blk)])
            if mix_cut <= 3:
                return
            A("pool", lambda e: e.tensor_copy(out=kTa[:, 0:128], in_=kTa[:, 512:640]), ["kT"], ["kTprev"])
            A("pool", lambda e: e.tensor_copy(out=vtok[:, 0, :], in_=vtok[:, 4, :]), ["vtok"], ["vprev"])

            if mix_cut <= 5:
                return
            if mix_cut <= 6:
                return
            gvk = [("gvtok", hv, bp) for hv in range(2) for bp in range(2)]
            ams = []
            mixk = [("mixA", b) for b in range(NBLK)] + [("mixG", b) for b in range(NBLK)]
            mixak = [("mixA", b) for b in range(NBLK)]
            WO_BANKS = [1, 3]
            OT_BANKS = [2, 0, 4, 5]
            wo_state = {}

            def wout_pass_a():
                wo_state["p"] = [W.acquire()]
                for dc in range(2):
                    pwa, pwak = wo_state["p"][dc // 2]
                    pwa3 = pwa.rearrange("p (k c) -> p k c", k=8)
                    b = WO_BANKS[dc]
                    dl = dc % 2
                    for kc in range(4):
                        MM(PS[b][:], pwa3[:, kc, dl * 128:(dl + 1) * 128], mixT[:, kc, :], kc == 0, False,
                           [pwak] + mixak, [pk(b)])

            for blk in range(NBLK):
                bc = slice(blk * 128, (blk + 1) * 128)
                bev, bod = (2, 6) if blk % 2 == 0 else (4, 5)
                for h in range(4):
                    pr = slice(64 * (h % 2), 64 * (h % 2) + 64)
                    P = h // 2
                    bsc = bev if h % 2 == 0 else bod
                    MM(PS[bsc][:, P * 128:(P + 1) * 128], gkT[pr, P, bc], gqT[pr, P, bc], True, True,
                       [("gkT", P), ("gqT", P)], [pk(bsc)])
                am, amk = rB.next()
                am4 = am.rearrange("p (a r q) -> p a r q", a=2, r=2)
                for r_ in range(2):
                    bsc = bev if r_ == 0 else bod
                    TTo("dve", am4[:, :, r_, :], PS[bsc][:, 0:256].rearrange("p (a q) -> p a q", a=2),
                        glamask[:, 0:256].rearrange("p (a q) -> p a q", a=2), ALU.mult, [pk(bsc), "cf"], [amk])
                ams.append((am, amk))
                bd = 3 if blk < 2 else 7
                for h in range(4):
                    pr = slice(64 * (h % 2), 64 * (h % 2) + 64)
                    P = h // 2
                    o = (blk % 2) * 256 + P * 128
                    MM(PS[bd][pr, o:o + 128], ktok[:, blk, h * 64:(h + 1) * 64], gvtok[:, blk, h * 128:(h + 1) * 128],
                       True, True, ["ktok"] + gvk, [pk(bd)])
            def state_step(blk):
                bd = 3 if blk < 2 else 7
                o = (blk % 2) * 256
                TTo("dve", Ust[:], Tst[:], PS[bd][:, o:o + 256], ALU.add, ["Tst", pk(bd)], ["Ust"])
                for P in range(2):
                    el = E1[:, P, blk * 128 + 127: blk * 128 + 128]
                    TS("dve", Tbf4[:, blk + 1, P * 128:(P + 1) * 128], Ust[:, P * 128:(P + 1) * 128], el, ALU.mult,
                       ["Ust", ("E1", P)], [("Tbf", blk + 1)])
                    TS("dve", Tst[:, P * 128:(P + 1) * 128], Ust[:, P * 128:(P + 1) * 128], el, ALU.mult,
                       ["Ust", ("E1", P)], ["Tst"])

            def s2_mm(blk):
                bc = slice(blk * 128, (blk + 1) * 128)
                am, amk = ams[blk]
                bo = OT_BANKS[blk]
                for h in range(4):
                    pr = slice(64 * (h % 2), 64 * (h % 2) + 64)
                    P = h // 2
                    MM(PS[bo][:, h * 128:(h + 1) * 128], gvtok[:, blk, h * 128:(h + 1) * 128], am[:, h * 128:(h + 1) * 128],
                       True, False, [amk] + gvk, [pk(bo)])
                    MM(PS[bo][:, h * 128:(h + 1) * 128], Tbf4[pr, blk, P * 128:(P + 1) * 128], gqT[pr, P, bc],
                       False, True, [("Tbf", blk), ("gqT", P)], [pk(bo)])
                sq, sqk = (sqb[:, blk, :], ("sqb", blk)) if blk < 2 else (sqe[:, blk - 2, :], ("sqe", blk - 2))
                ACT(sq, PS[bo][:], AF.Square, [pk(bo)], [sqk])
                return sq, sqk

            def s2_fin(blk, sqs):
                bc = slice(blk * 128, (blk + 1) * 128)
                sq, sqk = sqs
                bo = OT_BANKS[blk]
                bq = 7 if blk % 2 == 0 else 6
                MM(PS[bq][:], onesb, sq, True, True, [sqk, "cb"], [pk(bq)])
                rs, rsk = rF.next()
                ACT(rs, PS[bq][:], AF.Ln, [pk(bq)], [rsk], scale=1.0 / 128, bias=EPS)
                ACT(rs, rs, AF.Exp, [rsk], [rsk], scale=-0.5)
                og, ogk = rF.next()
                STT(og, PS[bo][:], col("gon"), rs, ALU.mult, ALU.mult, [pk(bo), rsk, "cols"], [ogk])
                TTo("dve", mixT[:, 4:8, bc], og.rearrange("p (h q) -> p h q", h=4), gsil[:, :, bc], ALU.mult,
                    [ogk] + [("gsil", h) for h in range(4)], [("mixG", blk)])

            sqs = []
            for blk in range(NBLK):
                state_step(blk)
                sqs.append(s2_mm(blk))
            for blk in range(NBLK - 1):
                s2_fin(blk, sqs[blk])
            wout_pass_a()
            s2_fin(NBLK - 1, sqs[NBLK - 1])
            A("pool", lambda e: e.tensor_copy(out=Tbf4[:, 0, :], in_=Tbf4[:, 4, :]), [("Tbf", 4)], [("Tbf", 0)])
            if mix_cut <= 7:
                return
            for i in range(1, 4):
                pw, pwk = W.acquire()
                pw3 = pw.rearrange("p (k c) -> p k c", k=8)
                if i == 1:
                    for dc in range(2):
                        pwa, pwak = wo_state["p"][dc // 2]
                        pwa3 = pwa.rearrange("p (k c) -> p k c", k=8)
                        b = WO_BANKS[dc]
                        dl = dc % 2
                        for kc in range(4, KC):
                            MM(PS[b][:], pwa3[:, kc, dl * 128:(dl + 1) * 128], mixT[:, kc, :], False, kc == KC - 1,
                               [pwak] + mixk, [pk(b)])
                        TTo("dve", hT[:, dc, :], PS[b][:], hT[:, dc, :], ALU.add, [pk(b), ("hT", dc)], [("hT", dc)])
                    W.release(1)
                for dl in range(2):
                    dc = 2 * i + dl
                    b = 4 + dl
                    for kc in range(KC):
                        MM(PS[b][:], pw3[:, kc, dl * 128:(dl + 1) * 128], mixT[:, kc, :], kc == 0, kc == KC - 1,
                           [pwk] + mixk, [pk(b)])
                    TTo("dve", hT[:, dc, :], PS[b][:], hT[:, dc, :], ALU.add, [pk(b), ("hT", dc)], [("hT", dc)])
                W.release()

        ple_state = {}

        def ple_emb_step(i):
            if i == 0:
                DMA("pool", ppbuf[:], w_d[n_pieces - 1], (), ["ppbuf"], lane="pp")
                ple_state["pp"] = (ppbuf[:], "ppbuf")
            if i in (0, 1):
                k2 = i
                b = 7
                for blk in range(NBLK):
                    TR(PS[b][:, blk * 128:(blk + 1) * 128], pin[:, blk, k2 * 128:(k2 + 1) * 128], ident, [("pin", blk), "cf"], [pk(b)])
                A("act", lambda e, b=b, k2=k2: e.copy(pT[:, k2, :], PS[b][:]), [pk(b)], [("pT", k2)])
                return
            pp, ppk = ple_state["pp"]
            pp3 = pp.rearrange("p (k c) -> p k c", k=2)
            if 2 <= i <= 9:
                dc = i - 2
                b = 7
                for k2 in range(2):
                    MM(PS[b][:], pp3[:, k2, dc * 128:(dc + 1) * 128], pT[:, k2, :], k2 == 0, k2 == 1, [ppk, ("pT", k2)], [pk(b)])
                A("act", lambda e, b=b, dc=dc: e.copy(embF[:, dc, :], PS[b][:]), [pk(b)], [("embF", dc)])
                sq, sqk = sqe[:, dc % 2, :], ("sqe", dc % 2)
                ACT(sq, PS[b][:], AF.Square, [pk(b)], [sqk])
                ple_state["sq%d" % dc] = (sq, sqk)
            if 3 <= i <= 10:
                dc = i - 3
                sq, sqk = ple_state["sq%d" % dc]
                MM(PS[6][:], onesb, sq, dc == 0, dc == KC - 1, [sqk, "cb"], [pk(6)])
            if i == 10:
                rs, rsk = c16[:, 0, :], ("c16", 0)
                ACT(rs, PS[6][:], AF.Ln, [pk(6)], [rsk], scale=1.0 / D, bias=EPS)
                ACT(rs, rs, AF.Exp, [rsk], [rsk], scale=-0.5)
                for dc in range(KC):
                    STT(embF[:, dc, :], embF[:, dc, :], col("npl", dc), rs, ALU.mult, ALU.mult,
                        [("embF", dc), rsk, "cols"], [("embF", dc)])

        def ple(t):
            norm_to_nT("npg")
            ems = [(embF[:, dc, :], ("embF", dc)) for dc in range(KC)]
            for i4 in range(4):
                pg, pgk2 = W.acquire()
                pg3 = pg.rearrange("p (k c) -> p k c", k=8)
                if i4 == 0:
                    for kc in range(KC):
                        for dl in range(2):
                            MM(PS[4 + dl][:], pg3[:, kc, dl * 128:(dl + 1) * 128], nT[:, kc, :], kc == 0, kc == KC - 1,
                               [pgk2, ("nT", kc)], [pk(4 + dl)])
                for dl in range(2):
                    dc = 2 * i4 + dl
                    b = 4 + dl
                    for kc in range(KC if i4 > 0 else 0):
                        MM(PS[b][:], pg3[:, kc, dl * 128:(dl + 1) * 128], nT[:, kc, :], kc == 0, kc == KC - 1,
                           [pgk2, ("nT", kc)], [pk(b)])
                    gt, gtk = rF.next()
                    ACT(gt, PS[b][:], AF.Sigmoid, [pk(b)], [gtk])
                    em, emk = ems[dc]
                    TTo("dve", em, em, gt, ALU.mult, [emk, gtk], [emk])
                    TTo("dve", hT[:, dc, :], hT[:, dc, :], em, ALU.add, [emk, ("hT", dc)], [("hT", dc)])
                W.release()

        for t in range(n_tiles):
            load_tile(t)
            if stop_after != "load":
                ffn("n1")
            if stop_after not in ("load", "ffn1"):
                mixer(t)
            if stop_after not in ("load", "ffn1", "mix"):
                ffn("n2", extra=ple_emb_step if stop_after is None else None)
            if stop_after not in ("load", "ffn1", "mix", "ffn2"):
                ple(t)
            store_tile(t)
        S.emit(final_waits=out_ops[-4:])
    return nc


_CACHE = {}


def kernel(**inputs):
    packed = _host_pack(inputs)
    x = np.asarray(inputs["x"], np.float32)
    p = np.asarray(inputs["p"], np.float32)[0]
    B = x.shape[0]
    if "nc" not in _CACHE:
        _CACHE["nc"] = build_nc()
    nc = _CACHE["nc"]
    in_maps = []
    for b in range(B):
        m = dict(packed)
        m["x"] = np.ascontiguousarray(x[b])
        m["p"] = np.ascontiguousarray(p[b])
        in_maps.append(m)
    res = run_bass_kernel_spmd(nc, in_maps, core_ids=list(range(B)))
    return np.stack([np.asarray(r["out"], np.float32) for r in res.results], 0)
```
